# Optimizing a Trainium2 kernel written in Bass

```python
import math
import jax
import jax.numpy as jnp
from jax import lax
import numpy as np

D_MODEL = 1024
BATCH = 8
SEQ = 4096
DEPTH = 2

GRID_W = 64
CTX_LEN = 256
EPS = 1e-6
N_AB = (DEPTH + 1) // 2
N_RET = DEPTH // 2

GLA_HEADS = 4
GLA_DK = 64
GLA_DV = 128
GLA_RANK = 16
GLA_TAU = 16.0
GLA_CHUNK = 64
AB_QK = GLA_HEADS * GLA_DK
AB_V = GLA_HEADS * GLA_DV
S5_CH = D_MODEL // 2
S5_GROUP = 16
S5_GROUPS = S5_CH // S5_GROUP
S5_P = 64
AB_IN = 2 * AB_QK + 2 * AB_V + 2 * GLA_RANK + S5_CH
AB_MIX = AB_V + S5_CH
RET_HEADS = 4
RET_DK = D_MODEL // RET_HEADS
RET_DV = 2 * RET_DK
RET_CHUNK = 128
RET_QK = RET_HEADS * RET_DK
RET_MIX = RET_HEADS * RET_DV
RET_IN = 2 * RET_QK + 2 * RET_MIX
ROPE_BASE = 10000.0
N_EXPERTS = 32
TOP_K = 4
D_FF = 1024
SWIGLU_LIMIT = 7.0
SWIGLU_ALPHA = 1.702
MOE_BLOCK = 256

kernel_name = 'hybrid_gla_s5_retention_moe_dit'


def rms_norm(x, g):
    xf = x.astype(jnp.float32)
    y = xf * lax.rsqrt(jnp.mean(xf * xf, axis=-1, keepdims=True) + EPS)
    return y * g.astype(jnp.float32)


def modulate(h, shift, scale):
    return h * (1.0 + scale[:, None, :]) + shift[:, None, :]


def chunk_gated_recurrence(q, k, v, log_a, s0, chunk, with_output):
    n = q.shape[2] // chunk

    def blocks(t):
        t = t.astype(jnp.float32)
        return t.reshape(t.shape[0], t.shape[1], n, chunk, t.shape[-1])

    qc, kc, vc = blocks(q), blocks(k), blocks(v)
    b = jnp.cumsum(blocks(log_a), axis=3)
    b_last = b[:, :, :, -1:, :]
    k_state = kc * jnp.exp(b_last - b)
    to_scan = lambda t: jnp.moveaxis(t, 2, 0)
    if with_output:
        q_dec = qc * jnp.exp(b)
        k_inv = kc * jnp.exp(-b)
        lower = jnp.tril(jnp.ones((chunk, chunk), dtype=bool))
        scores = jnp.where(lower, jnp.einsum('bhnik,bhnjk->bhnij', q_dec, k_inv), 0.0)
        o_intra = jnp.einsum('bhnij,bhnjv->bhniv', scores, vc)

        def step(s, xs):
            q_b, k_b, v_b, d_b = xs
            o_b = jnp.einsum('bhik,bhkv->bhiv', q_b, s)
            s = s * jnp.exp(jnp.swapaxes(d_b, -1, -2)) + jnp.einsum('bhjk,bhjv->bhkv', k_b, v_b)
            return s, o_b

        s_fin, o_inter = lax.scan(step, s0, (to_scan(q_dec), to_scan(k_state), to_scan(vc), to_scan(b_last)))
        o = o_intra + jnp.moveaxis(o_inter, 0, 2)
        return o.reshape(o.shape[0], o.shape[1], n * chunk, o.shape[-1]), s_fin

    def step_state(s, xs):
        k_b, v_b, d_b = xs
        return s * jnp.exp(jnp.swapaxes(d_b, -1, -2)) + jnp.einsum('bhjk,bhjv->bhkv', k_b, v_b), None

    s_fin, _ = lax.scan(step_state, s0, (to_scan(k_state), to_scan(vc), to_scan(b_last)))
    return None, s_fin


def prefix_recurrence(ctx_in, lat_in, chunk, reverse, ctx_out):
    flip = (lambda t: jnp.flip(t, axis=2)) if reverse else (lambda t: t)
    q_c, k_c, v_c, a_c = (flip(t) for t in ctx_in)
    q_l, k_l, v_l, a_l = (flip(t) for t in lat_in)
    s0 = jnp.zeros((k_c.shape[0], k_c.shape[1], k_c.shape[-1], v_c.shape[-1]), jnp.float32)
    o_c, s_c = chunk_gated_recurrence(q_c, k_c, v_c, a_c, s0, chunk, ctx_out)
    o_l, _ = chunk_gated_recurrence(q_l, k_l, v_l, a_l, s_c, chunk, True)
    return (flip(o_c) if ctx_out else None), flip(o_l)


def gla_heads(q, k, v, low, wa, ba):
    b, n, _ = q.shape

    def heads(t, dh):
        return t.reshape(b, n, GLA_HEADS, dh).transpose(0, 2, 1, 3)

    lows = jnp.split(low, 2, axis=-1)
    log_a = [heads(jax.nn.log_sigmoid((lows[d] @ wa[d] + ba[d]).astype(jnp.float32)) / GLA_TAU, GLA_DK)
             for d in range(2)]
    return heads(q, GLA_DK) * GLA_DK ** -0.5, heads(k, GLA_DK), heads(v, GLA_DV), log_a


def gla_output(o, g, norm_g):
    b, h, n, dv = o.shape
    o = o * lax.rsqrt(jnp.mean(o * o, axis=-1, keepdims=True) + EPS) * norm_g.astype(jnp.float32)
    return o.transpose(0, 2, 1, 3).reshape(b, n, h * dv) * jax.nn.silu(g.astype(jnp.float32))


def zoh(lam_re, lam_im, log_step):
    step = jnp.exp(log_step.astype(jnp.float32))[:, None]
    lam_re, lam_im = lam_re.astype(jnp.float32), lam_im.astype(jnp.float32)
    mag = jnp.exp(lam_re * step)
    lb_re, lb_im = mag * jnp.cos(lam_im * step), mag * jnp.sin(lam_im * step)
    den = lam_re * lam_re + lam_im * lam_im
    f_re = ((lb_re - 1.0) * lam_re + lb_im * lam_im) / den
    f_im = (lb_im * lam_re - (lb_re - 1.0) * lam_im) / den
    return lb_re, lb_im, f_re, f_im


def complex_diag_scan(a_re, a_im, b_re, b_im):
    n = b_re.shape[0]
    a_re = jnp.broadcast_to(a_re[None, None], (n, 1) + a_re.shape)
    a_im = jnp.broadcast_to(a_im[None, None], (n, 1) + a_im.shape)

    def combine(e1, e2):
        a1r, a1i, b1r, b1i = e1
        a2r, a2i, b2r, b2i = e2
        return (a2r * a1r - a2i * a1i, a2r * a1i + a2i * a1r,
                a2r * b1r - a2i * b1i + b2r, a2r * b1i + a2i * b1r + b2i)

    _, _, s_re, s_im = lax.associative_scan(combine, (a_re, a_im, b_re, b_im), axis=0)
    return s_re, s_im


def s5_bidirectional(u_ctx, u_lat, lam_re, lam_im, log_step, b_re, b_im, c_re, c_im, d_skip, glu_w, glu_b, ctx_out):
    def groups(u):
        return u.astype(jnp.float32).reshape(u.shape[0], u.shape[1], S5_GROUPS, S5_GROUP)

    uc, ul = groups(u_ctx), groups(u_lat)
    y_c, y_l = [], []
    for d in range(2):
        lb_re, lb_im, f_re, f_im = zoh(lam_re[d], lam_im[d], log_step[d])
        bb_re = f_re[..., None] * b_re - f_im[..., None] * b_im
        bb_im = f_re[..., None] * b_im + f_im[..., None] * b_re

        def drive(u):
            br = jnp.einsum('blgc,gpc->lbgp', u, bb_re)
            bi = jnp.einsum('blgc,gpc->lbgp', u, bb_im)
            return (br[::-1], bi[::-1]) if d == 1 else (br, bi)

        def readout(s_re, s_im):
            y = jnp.einsum('lbgp,gcp->blgc', s_re, c_re[d]) - jnp.einsum('lbgp,gcp->blgc', s_im, c_im[d])
            return y[:, ::-1] if d == 1 else y

        sc_re, sc_im = complex_diag_scan(lb_re, lb_im, *drive(uc))
        bl_re, bl_im = drive(ul)
        bl_re = bl_re.at[0].add(lb_re * sc_re[-1] - lb_im * sc_im[-1])
        bl_im = bl_im.at[0].add(lb_re * sc_im[-1] + lb_im * sc_re[-1])
        sl_re, sl_im = complex_diag_scan(lb_re, lb_im, bl_re, bl_im)
        y_l.append(readout(sl_re, sl_im))
        if ctx_out:
            y_c.append(readout(sc_re, sc_im))

    def finish(ys, u):
        y = ys[0] + ys[1] + d_skip.astype(jnp.float32).reshape(S5_GROUPS, S5_GROUP) * u
        y = jax.nn.gelu(y.reshape(y.shape[0], y.shape[1], S5_CH))
        return y * jax.nn.sigmoid(y @ glu_w + glu_b)

    return (finish(y_c, uc) if ctx_out else None), finish(y_l, ul)


def mixer_gla_s5(h_ctx, h_lat, w_in, w_out, gla_wa, gla_ba, gla_norm_g, lam_re, lam_im, log_step,
                 b_re, b_im, c_re, c_im, d_skip, glu_w, glu_b, ctx_out):
    split_at = [AB_QK, 2 * AB_QK, 2 * AB_QK + AB_V, 2 * AB_QK + 2 * AB_V, 2 * AB_QK + 2 * AB_V + 2 * GLA_RANK]
    q_c, k_c, v_c, g_c, low_c, u_c = jnp.split(h_ctx @ w_in, split_at, axis=-1)
    q_l, k_l, v_l, g_l, low_l, u_l = jnp.split(h_lat @ w_in, split_at, axis=-1)
    qc, kc, vc, la_c = gla_heads(q_c, k_c, v_c, low_c, gla_wa, gla_ba)
    ql, kl, vl, la_l = gla_heads(q_l, k_l, v_l, low_l, gla_wa, gla_ba)
    outs = [prefix_recurrence((qc, kc, vc, la_c[d]), (ql, kl, vl, la_l[d]), GLA_CHUNK, d == 1, ctx_out)
            for d in range(2)]
    s5_c, s5_l = s5_bidirectional(u_c, u_l, lam_re, lam_im, log_step, b_re, b_im, c_re, c_im,
                                  d_skip, glu_w, glu_b, ctx_out)
    y_lat = jnp.concatenate([gla_output(outs[0][1] + outs[1][1], g_l, gla_norm_g), s5_l], axis=-1) @ w_out
    y_ctx = None
    if ctx_out:
        y_ctx = jnp.concatenate([gla_output(outs[0][0] + outs[1][0], g_c, gla_norm_g), s5_c], axis=-1) @ w_out
    return y_ctx, y_lat


def axial_rope(t, row, col):
    half = t.shape[-1] // 2
    n_freq = half // 2
    inv_freq = ROPE_BASE ** (-jnp.arange(n_freq, dtype=jnp.float32) / n_freq)

    def rotate(tp, pos):
        ang = pos.astype(jnp.float32)[:, None] * inv_freq
        cos, sin = jnp.cos(ang)[None, :, None, :], jnp.sin(ang)[None, :, None, :]
        t1, t2 = tp[..., :n_freq], tp[..., n_freq:]
        return jnp.concatenate([t1 * cos - t2 * sin, t1 * sin + t2 * cos], axis=-1)

    return jnp.concatenate([rotate(t[..., :half], row), rotate(t[..., half:], col)], axis=-1)


def mixer_retention(h_ctx, h_lat, w_in, w_out, decay_logit, norm_g, row, col, ctx_out):
    split_at = [RET_QK, 2 * RET_QK, 2 * RET_QK + RET_MIX]

    def prep(h, rope):
        b, n, _ = h.shape
        q, k, v, g = jnp.split(h @ w_in, split_at, axis=-1)
        q = q.reshape(b, n, RET_HEADS, RET_DK)
        k = k.reshape(b, n, RET_HEADS, RET_DK)
        v = v.reshape(b, n, RET_HEADS, RET_DV)
        if rope:
            q, k = axial_rope(q, row, col), axial_rope(k, row, col)
        k = k * RET_DK ** -0.5
        return q.transpose(0, 2, 1, 3), k.transpose(0, 2, 1, 3), v.transpose(0, 2, 1, 3), g

    qc, kc, vc, gc = prep(h_ctx, False)
    ql, kl, vl, gl = prep(h_lat, True)
    log_gamma = jax.nn.log_sigmoid(decay_logit.astype(jnp.float32))

    def decay(d, n):
        return jnp.broadcast_to(log_gamma[d][None, :, None, None], (1, RET_HEADS, n, 1))

    outs = [prefix_recurrence((qc, kc, vc, decay(d, qc.shape[2])), (ql, kl, vl, decay(d, ql.shape[2])),
                              RET_CHUNK, d == 1, ctx_out) for d in range(2)]

    def out(o, g):
        b, h, n, dv = o.shape
        mu = jnp.mean(o, axis=-1, keepdims=True)
        var = jnp.mean(jnp.square(o - mu), axis=-1, keepdims=True)
        o = ((o - mu) * lax.rsqrt(var + EPS)).transpose(0, 2, 1, 3).reshape(b, n, h * dv)
        return (o * norm_g.astype(jnp.float32) * jax.nn.silu(g.astype(jnp.float32))) @ w_out

    y_lat = out(outs[0][1] + outs[1][1], gl)
    y_ctx = out(outs[0][0] + outs[1][0], gc) if ctx_out else None
    return y_ctx, y_lat


def moe_ffn(h, w_router, b_router, w_gu, b_gu, w_down, b_down):
    n, d = h.shape
    logits = (h @ w_router + b_router).astype(jnp.float32)
    top_logit, top_e = lax.top_k(logits, TOP_K)
    weights = jax.nn.softmax(top_logit, axis=-1)
    flat_e = top_e.reshape(-1)
    order = jnp.argsort(flat_e)
    e_sorted = flat_e[order]
    counts = jnp.bincount(flat_e, length=N_EXPERTS)
    padded = (counts + MOE_BLOCK - 1) // MOE_BLOCK * MOE_BLOCK
    pad_end = jnp.cumsum(padded)
    pad_start = pad_end - padded
    start = jnp.cumsum(counts) - counts
    dest = pad_start[e_sorted] + jnp.arange(n * TOP_K, dtype=jnp.int32) - start[e_sorted]
    n_blocks = (n * TOP_K + MOE_BLOCK - 1) // MOE_BLOCK + N_EXPERTS
    slot_tok = jnp.full((n_blocks * MOE_BLOCK,), n, jnp.int32).at[dest].set((order // TOP_K).astype(jnp.int32))
    slot_w = jnp.zeros((n_blocks * MOE_BLOCK,), jnp.float32).at[dest].set(weights.reshape(-1)[order])
    block_e = jnp.minimum(jnp.searchsorted(pad_end, jnp.arange(n_blocks, dtype=jnp.int32) * MOE_BLOCK,
                                           side='right'), N_EXPERTS - 1)
    h_pad = jnp.concatenate([h, jnp.zeros((1, d), h.dtype)], axis=0)
    xb = h_pad[slot_tok].reshape(n_blocks, MOE_BLOCK, d)

    def expert_block(args):
        x_blk, e = args
        gu = x_blk @ w_gu[e] + b_gu[e]
        gate = jnp.minimum(gu[:, :D_FF], SWIGLU_LIMIT)
        lin = jnp.clip(gu[:, D_FF:], -SWIGLU_LIMIT, SWIGLU_LIMIT)
        act = gate * jax.nn.sigmoid(SWIGLU_ALPHA * gate) * (lin + 1.0)
        return act @ w_down[e] + b_down[e]

    yb = lax.map(expert_block, (xb, block_e)).reshape(-1, d)
    y = jnp.zeros((n + 1, d), yb.dtype).at[slot_tok].add(yb * slot_w[:, None].astype(yb.dtype))
    return y[:n]


def setup_inputs(seed: int = 0) -> dict:
    key = jax.random.key(seed)
    keys = iter(jax.random.split(key, 64))
    f32 = jnp.float32

    def nrm(shape, scale):
        return scale * jax.random.normal(next(keys), shape, f32)

    def gain(shape):
        return 1.0 + nrm(shape, 0.02)

    lam_re = -0.5 + nrm((N_AB, 2, S5_GROUPS, S5_P), 0.01)
    lam_im = math.pi * jnp.arange(S5_P, dtype=f32) + nrm((N_AB, 2, S5_GROUPS, S5_P), 0.01)
    log_step = jax.random.uniform(next(keys), (N_AB, 2, S5_GROUPS), f32, math.log(1e-3), math.log(1e-1))
    ret_logit = jnp.log(2.0 ** (5.0 + jnp.arange(RET_HEADS, dtype=f32)) - 1.0) + nrm((N_RET, 2, RET_HEADS), 0.01)
    return {
        'x': nrm((BATCH, SEQ, D_MODEL), 1.0),
        'c': nrm((BATCH, D_MODEL), 1.0),
        'ctx': nrm((BATCH, CTX_LEN, D_MODEL), 1.0),
        'c_ctx': nrm((D_MODEL,), 1.0),
        'ada_w': nrm((DEPTH, D_MODEL, 6 * D_MODEL), 0.5 * D_MODEL ** -0.5),
        'ada_b': nrm((DEPTH, 6 * D_MODEL), 0.02),
        'norm1_g': gain((DEPTH, D_MODEL)),
        'norm2_g': gain((DEPTH, D_MODEL)),
        'ab_w_in': nrm((N_AB, D_MODEL, AB_IN), D_MODEL ** -0.5),
        'ab_w_out': nrm((N_AB, AB_MIX, D_MODEL), AB_MIX ** -0.5),
        'gla_wa': nrm((N_AB, 2, GLA_RANK, AB_QK), GLA_RANK ** -0.5),
        'gla_ba': nrm((N_AB, 2, AB_QK), 0.01),
        'gla_norm_g': gain((N_AB, GLA_DV)),
        's5_lam_re': lam_re,
        's5_lam_im': lam_im,
        's5_log_step': log_step,
        's5_b_re': nrm((N_AB, S5_GROUPS, S5_P, S5_GROUP), (2 * S5_GROUP) ** -0.5),
        's5_b_im': nrm((N_AB, S5_GROUPS, S5_P, S5_GROUP), (2 * S5_GROUP) ** -0.5),
        's5_c_re': nrm((N_AB, 2, S5_GROUPS, S5_GROUP, S5_P), (2 * S5_P) ** -0.5),
        's5_c_im': nrm((N_AB, 2, S5_GROUPS, S5_GROUP, S5_P), (2 * S5_P) ** -0.5),
        's5_d': nrm((N_AB, S5_CH), 1.0),
        's5_glu_w': nrm((N_AB, S5_CH, S5_CH), S5_CH ** -0.5),
        's5_glu_b': nrm((N_AB, S5_CH), 0.01),
        'ret_w_in': nrm((N_RET, D_MODEL, RET_IN), D_MODEL ** -0.5),
        'ret_w_out': nrm((N_RET, RET_MIX, D_MODEL), RET_MIX ** -0.5),
        'ret_decay_logit': ret_logit,
        'ret_norm_g': gain((N_RET, RET_MIX)),
        'moe_w_router': nrm((DEPTH, D_MODEL, N_EXPERTS), D_MODEL ** -0.5),
        'moe_b_router': nrm((DEPTH, N_EXPERTS), 0.01),
        'moe_w_gu': nrm((DEPTH, N_EXPERTS, D_MODEL, 2 * D_FF), D_MODEL ** -0.5),
        'moe_b_gu': nrm((DEPTH, N_EXPERTS, 2 * D_FF), 0.01),
        'moe_w_down': nrm((DEPTH, N_EXPERTS, D_FF, D_MODEL), D_FF ** -0.5),
        'moe_b_down': nrm((DEPTH, N_EXPERTS, D_MODEL), 0.01),
        'final_norm_g': gain((D_MODEL,)),
    }


def reference(x, c, ctx, c_ctx, ada_w, ada_b, norm1_g, norm2_g,
              ab_w_in, ab_w_out, gla_wa, gla_ba, gla_norm_g,
              s5_lam_re, s5_lam_im, s5_log_step, s5_b_re, s5_b_im, s5_c_re, s5_c_im,
              s5_d, s5_glu_w, s5_glu_b,
              ret_w_in, ret_w_out, ret_decay_logit, ret_norm_g,
              moe_w_router, moe_b_router, moe_w_gu, moe_b_gu, moe_w_down, moe_b_down,
              final_norm_g):
    batch, n_lat, d = x.shape
    n_ctx = ctx.shape[1]
    rows = n_lat // GRID_W
    row = jnp.broadcast_to(jnp.arange(rows, dtype=jnp.int32)[:, None], (rows, GRID_W)).reshape(-1)
    col = jnp.broadcast_to(jnp.arange(GRID_W, dtype=jnp.int32)[None, :], (rows, GRID_W)).reshape(-1)
    x_lat, x_ctx = x, ctx
    for layer in range(DEPTH):
        ctx_out = layer < DEPTH - 1
        i = layer // 2
        mod_lat = (jax.nn.silu(c) @ ada_w[layer] + ada_b[layer]).astype(jnp.float32)
        mod_ctx = (jax.nn.silu(c_ctx)[None, :] @ ada_w[layer] + ada_b[layer]).astype(jnp.float32)
        sh1_l, sc1_l, g1_l, sh2_l, sc2_l, g2_l = jnp.split(mod_lat, 6, axis=-1)
        sh1_c, sc1_c, g1_c, sh2_c, sc2_c, g2_c = jnp.split(mod_ctx, 6, axis=-1)
        h_lat = modulate(rms_norm(x_lat, norm1_g[layer]), sh1_l, sc1_l)
        h_ctx = modulate(rms_norm(x_ctx, norm1_g[layer]), sh1_c, sc1_c)
        if layer % 2 == 0:
            m_ctx, m_lat = mixer_gla_s5(h_ctx, h_lat, ab_w_in[i], ab_w_out[i], gla_wa[i], gla_ba[i], gla_norm_g[i],
                                        s5_lam_re[i], s5_lam_im[i], s5_log_step[i], s5_b_re[i], s5_b_im[i],
                                        s5_c_re[i], s5_c_im[i], s5_d[i], s5_glu_w[i], s5_glu_b[i], ctx_out)
        else:
            m_ctx, m_lat = mixer_retention(h_ctx, h_lat, ret_w_in[i], ret_w_out[i], ret_decay_logit[i],
                                           ret_norm_g[i], row, col, ctx_out)
        x_lat = (x_lat + g1_l[:, None, :] * m_lat).astype(x.dtype)
        h_lat = modulate(rms_norm(x_lat, norm2_g[layer]), sh2_l, sc2_l).reshape(-1, d)
        if ctx_out:
            x_ctx = (x_ctx + g1_c[:, None, :] * m_ctx).astype(ctx.dtype)
            h_ctx = modulate(rms_norm(x_ctx, norm2_g[layer]), sh2_c, sc2_c).reshape(-1, d)
            y = moe_ffn(jnp.concatenate([h_ctx, h_lat], axis=0), moe_w_router[layer], moe_b_router[layer],
                        moe_w_gu[layer], moe_b_gu[layer], moe_w_down[layer], moe_b_down[layer])
            x_ctx = (x_ctx + g2_c[:, None, :] * y[:batch * n_ctx].reshape(x_ctx.shape)).astype(ctx.dtype)
            y_lat = y[batch * n_ctx:]
        else:
            y_lat = moe_ffn(h_lat, moe_w_router[layer], moe_b_router[layer],
                            moe_w_gu[layer], moe_b_gu[layer], moe_w_down[layer], moe_b_down[layer])
        x_lat = (x_lat + g2_l[:, None, :] * y_lat.reshape(x_lat.shape)).astype(x.dtype)
    return rms_norm(x_lat, final_norm_g).astype(x.dtype)
```

```python
import contextlib
import math
import numpy as np
import concourse.bass as bass
import concourse.mybir as mybir
from concourse.bass_utils import run_bass_kernel_spmd

F32 = mybir.dt.float32
BF16 = mybir.dt.bfloat16
I32 = mybir.dt.int32
AF = mybir.ActivationFunctionType
ALU = mybir.AluOpType
AX = mybir.AxisListType

NT = 34
T = NT * 128
EPS = 1e-6


class Buf:
    __slots__ = ("name", "w", "r")

    def __init__(self, name):
        self.name = name
        self.w = None
        self.r = {}


class TT:
    def __init__(self, t, name):
        self.t = t
        self.name = name
        self.b = Buf(name)
        self.subs = {}

    def sub(self, key):
        if key not in self.subs:
            self.subs[key] = Buf(f"{self.name}[{key}]")
        return self.subs[key]

    def __getitem__(self, idx):
        return self.t[idx]


class FW:
    COMPUTE = ("pe", "dve", "act", "pool")
    NDMA = 8

    def __init__(self, nc):
        self.nc = nc
        self.es = contextlib.ExitStack()
        self.scopes = [self.es]
        self.prog = {k: [] for k in ("pe", "dve", "act", "pool", "sp")}
        self.sems = {}
        self.cnt = {}
        for k in self.COMPUTE:
            self.sems[k] = self.es.enter_context(nc.semaphore(f"s_{k}"))
            self.cnt[k] = 0
        self.dma_ring = {}
        for q in ("sp", "act", "pool"):
            ring = []
            for i in range(self.NDMA):
                key = f"d_{q}{i}"
                self.sems[key] = self.es.enter_context(nc.semaphore(key))
                self.cnt[key] = 0
                ring.append(key)
            self.dma_ring[q] = [ring, 0]
        self.seen = {k: {} for k in self.prog}
        self.n_inst = 0
        self.block = self.es.enter_context(nc.Block())
        self._uid = 0

    def sbuf(self, name, shape, dtype):
        self._uid += 1
        t = self.scopes[-1].enter_context(self.nc.sbuf_tensor(f"{name}_{self._uid}", list(shape), dtype))
        return TT(t, name)

    def psum(self, name, shape, dtype=F32):
        t = self.scopes[-1].enter_context(self.nc.psum_tensor(name, list(shape), dtype))
        return TT(t, name)

    def dram(self, name, shape, dtype):
        t = self.nc.dram_tensor(name, list(shape), dtype, kind="Internal")
        return TT(t.ap(), name)

    @contextlib.contextmanager
    def scope(self):
        st = contextlib.ExitStack()
        self.scopes.append(st)
        try:
            yield
        finally:
            self.barrier()
            self.flush()
            self.scopes.pop()
            st.close()

    @staticmethod
    def _bufs(xs):
        out = []
        for x in xs:
            if x is None:
                continue
            out.append(x.b if isinstance(x, TT) else x)
        return out

    def _deps(self, reads, writes):
        deps = {}

        def add(d):
            if d is None:
                return
            k, v = d
            if deps.get(k, 0) < v:
                deps[k] = v
        for b in reads:
            add(b.w)
        for b in writes:
            add(b.w)
            for k, v in b.r.items():
                add((k, v))
        return deps

    def _waits(self, eng, deps, own_key):
        waits = []
        seen = self.seen[eng]
        for k, v in deps.items():
            if k == "pe" and own_key == "pe":
                continue
            if seen.get(k, 0) >= v:
                continue
            seen[k] = v
            waits.append((self.sems[k], v))
        return waits

    def _mark(self, reads, writes, key, val):
        for b in reads:
            b.r[key] = val
        for b in writes:
            b.w = (key, val)
            b.r = {}

    def op(self, eng, fn, reads=(), writes=()):
        reads = self._bufs(reads)
        writes = self._bufs(writes)
        deps = self._deps(reads, writes)
        waits = self._waits(eng, deps, eng)
        self.cnt[eng] += 1
        val = self.cnt[eng]
        self.prog[eng].append((waits, fn, (self.sems[eng], 1)))
        self._mark(reads, writes, eng, val)
        self.n_inst += 1

    def dma(self, q, out, in_, reads=(), writes=(), **kw):
        reads = self._bufs(reads)
        writes = self._bufs(writes)
        ring, n = self.dma_ring[q]
        key = ring[n % len(ring)]
        self.dma_ring[q][1] = n + 1
        deps = self._deps(reads, writes)
        if self.cnt[key] > 0:
            deps[key] = max(deps.get(key, 0), self.cnt[key])
        waits = self._waits(q, deps, None)
        self.cnt[key] += 16
        val = self.cnt[key]
        self.prog[q].append((waits, lambda e: e.dma_start(out=out, in_=in_, **kw), (self.sems[key], 16)))
        self._mark(reads, writes, key, val)
        self.n_inst += 1

    def barrier(self):
        for eng in self.prog:
            waits = []
            for k, v in self.cnt.items():
                if v > 0 and self.seen[eng].get(k, 0) < v and not (k == eng):
                    waits.append((self.sems[k], v))
                    self.seen[eng][k] = v
            if waits:
                self.prog[eng].append((waits, None, None))

    def flush(self):
        fw = self
        names = {"sp": "sync", "pe": "tensor", "dve": "vector", "act": "scalar", "pool": "gpsimd"}
        for engk, items in self.prog.items():
            if not items:
                continue

            def body(e, items=items):
                for waits, fn, inc in items:
                    for s, v in waits:
                        e.wait_ge(s, v)
                    if fn is not None:
                        ins = fn(e)
                        ins.then_inc(inc[0], inc[1])
            getattr(self.block, names[engk])(body)
            self.prog[engk] = []

    def finish(self):
        self.barrier()
        self.flush()
        self.es.close()

    def mm(self, pst, out, lhsT, rhs, start=True, stop=True, reads=()):
        self.op("pe", lambda e: e.matmul(out, lhsT, rhs, start=start, stop=stop), reads=reads, writes=[pst])

    def tr(self, pst, out, in_, ident, reads=()):
        self.op("pe", lambda e: e.transpose(out, in_, ident), reads=reads, writes=[pst])

    def act(self, out, in_, func, reads, writes, **kw):
        self.op("act", lambda e: e.activation(out=out, in_=in_, func=func, **kw), reads=reads, writes=writes)

    def ts(self, eng, out, in0, s1, s2, op0, op1=None, reads=(), writes=(), **kw):
        if op1 is None:
            self.op(eng, lambda e: e.tensor_scalar(out=out, in0=in0, scalar1=s1, scalar2=s2, op0=op0, **kw), reads=reads, writes=writes)
        else:
            self.op(eng, lambda e: e.tensor_scalar(out=out, in0=in0, scalar1=s1, scalar2=s2, op0=op0, op1=op1, **kw), reads=reads, writes=writes)

    def stt(self, out, in0, scalar, in1, op0, op1, reads=(), writes=(), **kw):
        self.op("dve", lambda e: e.scalar_tensor_tensor(out=out, in0=in0, scalar=scalar, in1=in1, op0=op0, op1=op1, **kw), reads=reads, writes=writes)

    def tt(self, eng, out, in0, in1, op, reads=(), writes=()):
        self.op(eng, lambda e: e.tensor_tensor(out=out, in0=in0, in1=in1, op=op), reads=reads, writes=writes)

    def cp(self, eng, out, in_, reads=(), writes=()):
        if eng == "act":
            self.op("act", lambda e: e.activation(out=out, in_=in_, func=AF.Identity), reads=reads, writes=writes)
        else:
            self.op(eng, lambda e: e.tensor_copy(out=out, in_=in_), reads=reads, writes=writes)

    def memset(self, eng, ap, val, writes=()):
        self.op(eng, lambda e: e.memset(ap, val), writes=writes)


PARAMS = [
    ("ada_w", [2, 1024, 6144]), ("ada_b", [2, 6144]), ("norm1_g", [2, 1024]), ("norm2_g", [2, 1024]),
    ("ab_w_in", [1, 1024, 2080]), ("ab_w_out", [1, 1024, 1024]), ("gla_wa", [1, 2, 16, 256]), ("gla_ba", [1, 2, 256]),
    ("gla_norm_g", [1, 128]), ("s5_lam_re", [1, 2, 32, 64]), ("s5_lam_im", [1, 2, 32, 64]), ("s5_log_step", [1, 2, 32]),
    ("s5_b_re", [1, 32, 64, 16]), ("s5_b_im", [1, 32, 64, 16]), ("s5_c_re", [1, 2, 32, 16, 64]), ("s5_c_im", [1, 2, 32, 16, 64]),
    ("s5_d", [1, 512]), ("s5_glu_w", [1, 512, 512]), ("s5_glu_b", [1, 512]),
    ("ret_w_in", [1, 1024, 6144]), ("ret_w_out", [1, 2048, 1024]), ("ret_decay_logit", [1, 2, 4]), ("ret_norm_g", [1, 2048]),
    ("moe_w_router", [2, 1024, 32]), ("moe_b_router", [2, 32]), ("moe_w_gu", [2, 32, 1024, 2048]), ("moe_b_gu", [2, 32, 2048]),
    ("moe_w_down", [2, 32, 1024, 1024]), ("moe_b_down", [2, 32, 1024]), ("final_norm_g", [1024]),
]


def make_consts():
    j = np.arange(128)[:, None]
    t = np.arange(128)[None, :]
    cm = np.zeros((128, 8, 128), np.float32)
    cm[:, 0] = (j == t)
    cm[:, 1] = (j <= t)
    cm[:, 2] = (j >= t)
    cm[:, 3] = (j > t)
    cm[:, 4] = (j < t)
    cm[:, 5] = 1.0
    cm[:, 6] = t + 1.0
    cm[:, 7] = 128.0 - t
    s5m = np.zeros((128, 4, 128), np.float32)
    for q in range(4):
        s5m[:, q] = ((t // 16) == (2 * q + (j >= 64)))
    n_freq = 64
    inv_freq = (10000.0 ** (-np.arange(n_freq, dtype=np.float32) / n_freq)).astype(np.float32)
    pos_row = (np.arange(4096) // 64).astype(np.float32)
    pos_col = (np.arange(4096) % 64).astype(np.float32)
    rope = np.zeros((2, 2, 128, 4096), np.float32)
    for ty, pos in enumerate((pos_row, pos_col)):
        ang = (pos[None, :] * inv_freq[:, None]).astype(np.float32)
        c, s = np.cos(ang), np.sin(ang)
        rope[ty, 0, :64] = c
        rope[ty, 0, 64:] = c
        rope[ty, 1, :64] = -s
        rope[ty, 1, 64:] = s
    gm = np.zeros((128, 258), np.float32)
    gm[:64, 0] = 1.0
    gm[64:, 1] = 1.0
    gm[:, 2:] = ((np.arange(128)[:, None] // 64) == (np.arange(256)[None, :] // 128))
    return {"cm": cm, "s5m": s5m, "rope": rope, "gm": gm}


def build(upto=99, dbg=None):
    import os
    GSTEP = int(os.environ.get('GLA_STEP', '99'))
    nc = bass.Bass("TRN2", target_bir_lowering=False)

    def din(name, shape, dt=F32):
        return nc.dram_tensor(name, list(shape), dt, kind="ExternalInput").ap()

    shapes = {"xin": [T, 1024], "cvec": [128, 16], "cm": [128, 8, 128], "s5m": [128, 4, 128], "rope": [2, 2, 128, 4096], "gm": [128, 258]}
    shapes.update({n_: s_ for n_, s_ in PARAMS})

    class LazyIn(dict):
        def __missing__(self, name):
            self[name] = din(name, shapes[name])
            return self[name]
    I = LazyIn()
    yout = nc.dram_tensor("yout", [4096, 1024], F32, kind="ExternalOutput").ap()
    dbg_out = None
    if dbg is not None:
        dbg_out = nc.dram_tensor("dbg", list(dbg), F32, kind="ExternalOutput").ap()

    k = FW(nc)
    xin_T = TT(I["xin"], "xin")
    xres = k.dram("xres", [T, 1024], F32)
    ofD = k.dram("ofD", [T, 2048], F32)
    yfD = k.dram("yfD", [512, T], F32)
    mixT = k.dram("mixT", [2048, T], BF16)
    dbgT = TT(dbg_out, "dbg") if dbg_out is not None else None
    youtT = TT(yout, "yout")

    cmf = k.sbuf("cmf", [128, 8, 128], F32)
    k.dma("sp", cmf[:], I["cm"], writes=[cmf])
    cmb = k.sbuf("cmb", [128, 8, 128], BF16)
    k.cp("dve", cmb[:], cmf[:], reads=[cmf], writes=[cmb])
    identf, identb = cmf[:, 0, :], cmb[:, 0, :]
    onesf, onesb = cmf[:, 5, :], cmb[:, 5, :]
    PF = [k.psum(f"pf{i}", [128, 512], F32) for i in range(5)]
    PX = k.psum("px", [128, 512], F32)
    PB = [k.psum(f"pb{i}", [128, 1024], BF16) for i in range(2)]
    pcnt = [0, 0]

    def pf():
        pcnt[0] += 1
        return PF[pcnt[0] % 5]

    def pb():
        pcnt[1] += 1
        return PB[pcnt[1] % 2]

    hT = k.sbuf("hT", [128, 8, T], BF16)

    def hTi(kc, i, n=1):
        return hT[:, kc, i * 128:(i + n) * 128]

    def stage_mod(l, L):
        with k.scope():
            sc = k.sbuf("m_sc", [128, 16], F32)
            k.dma("sp", sc[:], I["cvec"], writes=[sc])
            k.act(sc[:], sc[:], AF.Silu, reads=[sc], writes=[sc])
            screp = k.sbuf("m_screp", [128, 16, 128], F32)
            for i in range(16):
                k.ts("dve", screp[:, i, :], onesf, sc[:, i:i + 1], None, ALU.mult, reads=[cmf, sc], writes=[screp])
            vst = k.sbuf("m_vst", [64, 128], F32)
            k.dma("sp", vst[0:48, :], I["ada_b"][l].rearrange("(j p) -> j p", p=128), writes=[vst])
            k.dma("sp", vst[48:56, :], I["norm1_g"][l].rearrange("(j p) -> j p", p=128), writes=[vst])
            k.dma("sp", vst[56:64, :], I["norm2_g"][l].rearrange("(j p) -> j p", p=128), writes=[vst])
            ps = pf()
            k.tr(ps, ps[:, 0:64], vst[:], identf[0:64, 0:64], reads=[vst, cmf])
            vecT = L["vecT"]
            k.cp("dve", vecT[:], ps[:, 0:64], reads=[ps], writes=[vecT])
            modF = L["modF"]
            adab = k.sbuf("m_adab", [1, 2048], F32)
            k.dma("sp", adab[0:1, 0:1024], I["ada_b"][l:l + 1, 2048:3072], writes=[adab])
            k.dma("sp", adab[0:1, 1024:2048], I["ada_b"][l:l + 1, 5120:6144], writes=[adab])
            wring = [k.sbuf(f"m_w{i}", [128, 8, 512], F32) for i in range(2)]
            awv = I["ada_w"][l].rearrange("(kc p) n -> p kc n", p=128)
            for blk in range(12):
                wt = wring[blk % 2]
                for kc in range(8):
                    k.dma("sp" if kc % 2 == 0 else "act", wt[:, kc, :], awv[:, kc, blk * 512:(blk + 1) * 512], writes=[wt])
                ps = pf()
                for jj in range(4):
                    for kc in range(8):
                        k.mm(ps, ps[:, jj * 2:jj * 2 + 2], wt[:, kc, jj * 128:(jj + 1) * 128], sc[:, kc::8],
                             start=(kc == 0), stop=(kc == 7), reads=[wt, sc])
                k.tt("dve", modF[:, blk * 4:(blk + 1) * 4, :], ps[:, 0:8].rearrange("p (a b) -> p a b", b=2),
                     vecT[:, blk * 4:(blk + 1) * 4].unsqueeze(2).to_broadcast([128, 4, 2]), ALU.add,
                     reads=[ps, vecT], writes=[modF])
                if blk in (4, 5, 10, 11):
                    which = 0 if blk < 6 else 1
                    half = blk % 2
                    for s in range(2):
                        p2 = pf()
                        for kc in range(8):
                            k.mm(p2, p2[:, 0:512], screp[:, s * 8 + kc, :], wt[:, kc, :], start=(kc == 0), stop=False, reads=[screp, wt])
                        k.mm(p2, p2[:, 0:512], onesf[0:1, :], adab[0:1, which * 1024 + half * 512: which * 1024 + (half + 1) * 512],
                             start=False, stop=True, reads=[cmf, adab])
                        g = L["grow"][which][s]
                        k.cp("act", g[:, half * 512:(half + 1) * 512], p2[:, 0:512], reads=[p2], writes=[g])
            for wi, (scb, ngb) in enumerate(((8, 48), (32, 56))):
                k.stt(L["mulA"][wi][:], modF[:, scb:scb + 8, :], 1.0, vecT[:, ngb:ngb + 8].unsqueeze(2).to_broadcast([128, 8, 2]),
                      ALU.add, ALU.mult, reads=[modF, vecT], writes=[L["mulA"][wi]])

    def pass_a(L, wi, tiles, src, dst_col0=0, hdst=None):
        hdst = hdst or hT
        shb = 0 if wi == 0 else 24
        with k.scope():
            xts = [k.sbuf(f"a_x{i}", [128, 1024], F32) for i in range(3)]
            junk = k.sbuf("a_junk", [128, 1024], BF16)
            xn = [k.sbuf(f"a_xn{i}", [128, 1024], BF16) for i in range(2)]
            ss = [k.sbuf(f"a_ss{i}", [128, 1], F32) for i in range(2)]
            for n, i in enumerate(tiles):
                s = 1 if i < 2 else 0
                xt = xts[n % 3]
                k.dma("sp", xt[:], src[i * 128:(i + 1) * 128, :], reads=[src.sub(i)], writes=[xt])
                s_ = ss[n % 2]
                k.act(junk[:], xt[:], AF.Square, reads=[xt], writes=[junk, s_], accum_out=s_[:])
                k.act(s_[:], s_[:], AF.Sqrt, reads=[s_], writes=[s_], scale=1.0 / 1024, bias=EPS)
                k.op("dve", lambda e, s_=s_: e.reciprocal(out=s_[:], in_=s_[:]), reads=[s_], writes=[s_])
                x_ = xn[n % 2]
                k.ts("dve", x_[:], xt[:], s_[:, 0:1], None, ALU.mult, reads=[xt, s_], writes=[x_])
                p = pb()
                for j in range(8):
                    k.tr(p, p[:, j * 128:(j + 1) * 128], x_[:, j * 128:(j + 1) * 128], identb, reads=[x_, cmb])
                c0 = dst_col0 + n * 128 if hdst is not hT else i * 128
                key = i if hdst is hT else n
                for j in range(8):
                    o = hdst[:, j, c0:c0 + 128]
                    if j % 2 == 0:
                        k.act(o, p[:, j * 128:(j + 1) * 128], AF.Identity, reads=[p, L["mulA"][wi], L["modF"]], writes=[hdst.sub(key)],
                              scale=L["mulA"][wi][:, j, s:s + 1], bias=L["modF"][:, shb + j, s:s + 1])
                    else:
                        k.ts("dve", o, p[:, j * 128:(j + 1) * 128], L["mulA"][wi][:, j, s:s + 1], L["modF"][:, shb + j, s:s + 1],
                             ALU.mult, ALU.add, reads=[p, L["mulA"][wi], L["modF"]], writes=[hdst.sub(key)])

    def hreads(i, n=1):
        return [hT.sub(i + a) for a in range(n)]

    def stage_gla():
        with k.scope():
            W = k.sbuf("g_W", [128, 8, 1568], BF16)
            for kc in range(8):
                k.dma("pool", W[:, kc, :], I["ab_w_in"][0, kc * 128:(kc + 1) * 128, 0:1568], writes=[W])
            wa2f = k.sbuf("g_wa2f", [32, 2, 256], F32)
            k.memset("dve", wa2f[:], 0.0, writes=[wa2f])
            k.dma("sp", wa2f[0:16, 0, :], I["gla_wa"][0, 0], writes=[wa2f])
            k.dma("sp", wa2f[16:32, 1, :], I["gla_wa"][0, 1], writes=[wa2f])
            wa2 = k.sbuf("g_wa2", [32, 2, 256], BF16)
            k.cp("dve", wa2[:], wa2f[:], reads=[wa2f], writes=[wa2])
            baf = k.sbuf("g_baf", [1, 2, 256], F32)
            k.dma("sp", baf[:], I["gla_ba"][0:1], writes=[baf])
            bab = k.sbuf("g_bab", [1, 2, 256], BF16)
            k.cp("dve", bab[:], baf[:], reads=[baf], writes=[bab])
            normg = k.sbuf("g_ng", [128, 128], F32)
            k.dma("sp", normg[:], I["gla_norm_g"][0].partition_broadcast(128), writes=[normg])
            gmf = k.sbuf("g_gm", [128, 258], F32)
            k.dma("sp", gmf[:], I["gm"], writes=[gmf])
            mask4 = [k.sbuf(f"g_mask{d}", [128, 4, 128], BF16) for d in range(2)]
            for d in range(2):
                for h in range(4):
                    k.cp("dve", mask4[d][:, h, :], cmf[:, 1 + d, :], reads=[cmf], writes=[mask4[d]])
            S32 = [k.sbuf(f"g_S32{c}", [128, 256], F32) for c in range(2)]
            Sbf = [k.sbuf(f"g_Sbf{c}", [128, 256], BF16) for c in range(2)]
            lowT = k.sbuf("g_lowT", [32, 128], BF16)
            e1 = k.sbuf("g_e1", [128, 256], F32)
            l1 = k.sbuf("g_l1", [128, 256], BF16)
            EpT = k.sbuf("g_EpT", [128, 256], F32)
            EmT = k.sbuf("g_EmT", [128, 256], F32)
            Ec = k.sbuf("g_Ec", [128, 256], F32)
            qdT = k.sbuf("g_qdT", [128, 256], BF16)
            kiT = k.sbuf("g_kiT", [128, 4, 128], BF16)
            kst = k.sbuf("g_kst", [128, 256], BF16)
            vb = k.sbuf("g_vb", [128, 512], BF16)
            ST = k.sbuf("g_ST", [128, 512], BF16)
            o32 = [k.sbuf(f"g_o32{i}", [128, 512], F32) for i in range(2)]
            of_in = [k.sbuf(f"g_ofin{i}", [128, 512], F32) for i in range(2)]
            sg = k.sbuf("g_sg", [128, 512], F32)
            junk = k.sbuf("g_junk", [128, 128], BF16)
            ss4 = k.sbuf("g_ss4", [128, 4], F32)
            fin = k.sbuf("g_fin", [128, 512], BF16)
            finT = [k.sbuf(f"g_finT{i}", [128, 4, 128], BF16) for i in range(2)]
            cnt = 0
            for d in range(2):
                order = list(range(NT)) if d == 0 else [1, 0] + list(range(NT - 1, 1, -1))

                if os.environ.get("GLA_LIM"):
                    order = order[:int(os.environ["GLA_LIM"])]
                    if d == 1 and os.environ.get("GLA_D0"):
                        continue
                tri = cmb[:, 1 + d, :]
                sx = cmb[:, 3 + d, :]
                last = 127 if d == 0 else 0
                for c in range(2):
                    k.memset("dve", S32[c][:], 0.0, writes=[S32[c]])
                    k.memset("dve", Sbf[c][:], 0.0, writes=[Sbf[c]])
                for i in order:
                    if GSTEP < 1:
                        continue
                    hr = hreads(i)
                    pq = pf()
                    for g4 in range(4):
                        for kc in range(8):
                            k.mm(pq, pq[:, g4 * 128:(g4 + 1) * 128], W[:, kc, g4 * 128:(g4 + 1) * 128], hTi(kc, i),
                                 start=(kc == 0), stop=(kc == 7), reads=[W] + hr)
                    pl = pf()
                    for kc in range(8):
                        k.mm(pl, pl[0:32, 0:128], W[:, kc, 1536:1568], hTi(kc, i), start=(kc == 0), stop=(kc == 7), reads=[W] + hr)
                    k.cp("act", lowT[:], pl[0:32, 0:128], reads=[pl], writes=[lowT])
                    if GSTEP < 2:
                        continue
                    pz = pf()
                    k.mm(pz, pz[:, 0:256], lowT[:], wa2[:, d, :], start=True, stop=False, reads=[lowT, wa2])
                    k.mm(pz, pz[:, 0:256], onesb[0:1, :], bab[0:1, d, :], start=False, stop=True, reads=[cmb, bab])
                    k.act(e1[:], pz[:, 0:256], AF.Exp, reads=[pz], writes=[e1], scale=-1.0)
                    k.act(l1[:], e1[:], AF.Ln, reads=[e1], writes=[l1], bias=1.0)
                    if GSTEP < 3:
                        continue
                    pbt = pf()
                    for c in range(2):
                        k.mm(pbt, pbt[:, c * 128:(c + 1) * 128], l1[:, c * 128:(c + 1) * 128], tri, reads=[l1, cmb])
                    k.act(EpT[:], pbt[:, 0:256], AF.Exp, reads=[pbt], writes=[EpT], scale=-1.0 / 16)
                    k.act(EmT[:], pbt[:, 0:256], AF.Exp, reads=[pbt], writes=[EmT], scale=1.0 / 16)
                    k.stt(qdT[:], pq[:, 0:256], 0.125, EpT[:], ALU.mult, ALU.mult, reads=[pq, EpT], writes=[qdT])
                    for h in range(4):
                        c, hh = h // 2, h % 2
                        k.stt(kiT[:, h, :], pq[:, 256 + c * 128:256 + (c + 1) * 128], gmf[:, hh:hh + 1], EmT[:, c * 128:(c + 1) * 128],
                              ALU.mult, ALU.mult, reads=[pq, gmf, EmT], writes=[kiT])
                    if GSTEP < 4:
                        continue
                    pk = pf()
                    pv = pf()
                    for kc in range(8):
                        k.mm(pk, pk[:, 0:256], hTi(kc, i), W[:, kc, 256:512], start=(kc == 0), stop=(kc == 7), reads=[W] + hr)
                    for kc in range(8):
                        k.mm(pv, pv[:, 0:512], hTi(kc, i), W[:, kc, 512:1024], start=(kc == 0), stop=(kc == 7), reads=[W] + hr)
                    pc = pf()
                    k.mm(pc, pc[:, 0:256], sx, l1[:], reads=[cmb, l1])
                    k.act(Ec[:], pc[:, 0:256], AF.Exp, reads=[pc], writes=[Ec], scale=-1.0 / 16)
                    k.tt("dve", kst[:], pk[:, 0:256], Ec[:], ALU.mult, reads=[pk, Ec], writes=[kst])
                    k.cp("act", vb[:], pv[:, 0:512], reads=[pv], writes=[vb])
                    if GSTEP < 5:
                        continue
                    pss = pf()
                    for h in range(4):
                        c = h // 2
                        k.mm(pss, pss[:, h * 128:(h + 1) * 128], kiT[:, h, :], qdT[:, c * 128:(c + 1) * 128], reads=[kiT, qdT])
                    k.tt("dve", ST[:], pss[:, 0:512], mask4[d][:].rearrange("p a b -> p (a b)"), ALU.mult, reads=[pss, mask4[d]], writes=[ST])
                    if GSTEP < 6:
                        continue
                    po = pf()
                    for h in range(4):
                        c, hh = h // 2, h % 2
                        k.mm(po, po[:, h * 128:(h + 1) * 128], qdT[:, c * 128:(c + 1) * 128], Sbf[c][:, hh * 128:(hh + 1) * 128],
                             start=True, stop=False, reads=[qdT, Sbf[c]])
                        k.mm(po, po[:, h * 128:(h + 1) * 128], ST[:, h * 128:(h + 1) * 128], vb[:, h * 128:(h + 1) * 128],
                             start=False, stop=True, reads=[ST, vb])
                    if GSTEP < 7:
                        continue
                    pst = pf()
                    for c in range(2):
                        k.mm(pst, pst[:, c * 256:(c + 1) * 256], kst[:, c * 128:(c + 1) * 128], vb[:, c * 256:(c + 1) * 256], reads=[kst, vb])
                    for c in range(2):
                        k.stt(S32[c][:], S32[c][:], EpT[:, c * 128 + last:c * 128 + last + 1], pst[:, c * 256:(c + 1) * 256], ALU.mult, ALU.add,
                              reads=[S32[c], EpT, pst], writes=[S32[c]])
                        k.tt("pool", S32[c][:], S32[c][:], gmf[:, 2:258], ALU.mult, reads=[S32[c], gmf], writes=[S32[c]])
                        k.cp("pool", Sbf[c][:], S32[c][:], reads=[S32[c]], writes=[Sbf[c]])
                    if GSTEP < 8:
                        continue
                    cnt += 1
                    if d == 0:
                        o_ = o32[cnt % 2]
                        k.cp("act", o_[:], po[:, 0:512], reads=[po], writes=[o_])
                        k.dma("sp", ofD[i * 128:(i + 1) * 128, 0:512], o_[:], reads=[o_], writes=[ofD.sub(i)])
                    else:
                        oi = of_in[cnt % 2]
                        k.dma("sp", oi[:], ofD[i * 128:(i + 1) * 128, 0:512], reads=[ofD.sub(i)], writes=[oi])
                        o_ = o32[cnt % 2]
                        k.tt("dve", o_[:], po[:, 0:512], oi[:], ALU.add, reads=[po, oi], writes=[o_])
                        for h in range(4):
                            k.act(junk[:], o_[:, h * 128:(h + 1) * 128], AF.Square, reads=[o_], writes=[junk, ss4], accum_out=ss4[:, h:h + 1])
                        k.act(ss4[:], ss4[:], AF.Sqrt, reads=[ss4], writes=[ss4], scale=1.0 / 128, bias=EPS)
                        k.op("dve", lambda e: e.reciprocal(out=ss4[:], in_=ss4[:]), reads=[ss4], writes=[ss4])
                        pg = pf()
                        for kc in range(8):
                            k.mm(pg, pg[:, 0:512], hTi(kc, i), W[:, kc, 1024:1536], start=(kc == 0), stop=(kc == 7), reads=[W] + hr)
                        k.act(sg[:], pg[:, 0:512], AF.Silu, reads=[pg], writes=[sg])
                        for h in range(4):
                            k.stt(o_[:, h * 128:(h + 1) * 128], o_[:, h * 128:(h + 1) * 128], ss4[:, h:h + 1], normg[:], ALU.mult, ALU.mult,
                                  reads=[o_, ss4, normg], writes=[o_])
                        k.tt("dve", fin[:], o_[:], sg[:], ALU.mult, reads=[o_, sg], writes=[fin])
                        p = pb()
                        for c in range(4):
                            k.tr(p, p[:, c * 128:(c + 1) * 128], fin[:, c * 128:(c + 1) * 128], identb, reads=[fin, cmb])
                        ft = finT[cnt % 2]
                        k.cp("act", ft[:].rearrange("p a b -> p (a b)"), p[:, 0:512], reads=[p], writes=[ft])
                        k.dma("sp", mixT[0:512, i * 128:(i + 1) * 128].rearrange("(c p) t -> p c t", p=128), ft[:],
                              reads=[ft], writes=[mixT.sub(("g", i))])

    def stage_s5():
        with k.scope():
            pst_ = k.sbuf("s_pst", [96, 128], F32)
            k.dma("sp", pst_[0:32, :], I["s5_lam_re"][0].rearrange("d (st gl) p -> (d st) (gl p)", gl=2), writes=[pst_])
            k.dma("sp", pst_[32:64, :], I["s5_lam_im"][0].rearrange("d (st gl) p -> (d st) (gl p)", gl=2), writes=[pst_])
            lsm = k.sbuf("s_lsm", [96, 2], F32)
            k.dma("sp", lsm[64:96, :], I["s5_log_step"][0].rearrange("d (st gl) -> (d st) gl", gl=2), writes=[lsm])
            for gl in range(2):
                k.cp("dve", pst_[64:96, gl * 64:(gl + 1) * 64], lsm[64:96, gl:gl + 1].to_broadcast([32, 64]), reads=[lsm], writes=[pst_])
            ps = pf()
            k.tr(ps, ps[:, 0:96], pst_[:], identf[0:96, 0:96], reads=[pst_, cmf])
            par = k.sbuf("s_par", [128, 96], F32)
            k.cp("dve", par[:], ps[:, 0:96], reads=[ps], writes=[par])
            lre, lim = par[:, 0:32], par[:, 32:64]
            names = ["step", "mag", "th", "y", "fr", "t1", "t2", "cb", "sb", "lbr", "lbi", "den", "fre", "fim", "rr", "nfim"]
            V = {n: k.sbuf("s_" + n, [128, 32], F32) for n in names}
            yi = k.sbuf("s_yi", [128, 32], I32)

            def dv(out, a, b, op):
                k.tt("dve", V[out][:], a, b, op, reads=[par] + list(V.values()), writes=[V[out]])

            k.act(V["step"][:], par[:, 64:96], AF.Exp, reads=[par], writes=[V["step"]])
            dv("t1", lre, V["step"][:], ALU.mult)
            k.act(V["mag"][:], V["t1"][:], AF.Exp, reads=[V["t1"]], writes=[V["mag"]])
            dv("th", lim, V["step"][:], ALU.mult)
            k.ts("dve", V["y"][:], V["th"][:], 1.0 / (2 * math.pi), None, ALU.mult, reads=[V["th"]], writes=[V["y"]])
            k.cp("dve", yi[:], V["y"][:], reads=[V["y"]], writes=[yi])
            k.cp("dve", V["t1"][:], yi[:], reads=[yi], writes=[V["t1"]])
            dv("fr", V["y"][:], V["t1"][:], ALU.subtract)

            def wrap(name):
                k.ts("dve", V["t1"][:], V[name][:], 0.5, None, ALU.is_gt, reads=[V[name]], writes=[V["t1"]])
                k.ts("dve", V["t2"][:], V[name][:], -0.5, None, ALU.is_lt, reads=[V[name]], writes=[V["t2"]])
                dv(name, V[name][:], V["t1"][:], ALU.subtract)
                dv(name, V[name][:], V["t2"][:], ALU.add)
            wrap("fr")
            k.act(V["sb"][:], V["fr"][:], AF.Sin, reads=[V["fr"]], writes=[V["sb"]], scale=2 * math.pi)
            k.ts("dve", V["fr"][:], V["fr"][:], 0.25, None, ALU.add, reads=[V["fr"]], writes=[V["fr"]])
            wrap("fr")
            k.act(V["cb"][:], V["fr"][:], AF.Sin, reads=[V["fr"]], writes=[V["cb"]], scale=2 * math.pi)
            dv("lbr", V["mag"][:], V["cb"][:], ALU.mult)
            dv("lbi", V["mag"][:], V["sb"][:], ALU.mult)
            dv("t1", lre, lre, ALU.mult)
            dv("t2", lim, lim, ALU.mult)
            dv("den", V["t1"][:], V["t2"][:], ALU.add)
            k.op("dve", lambda e: e.reciprocal(out=V["den"][:], in_=V["den"][:]), reads=[V["den"]], writes=[V["den"]])
            k.ts("dve", V["y"][:], V["lbr"][:], -1.0, None, ALU.add, reads=[V["lbr"]], writes=[V["y"]])
            dv("t1", V["y"][:], lre, ALU.mult)
            dv("t2", V["lbi"][:], lim, ALU.mult)
            dv("fre", V["t1"][:], V["t2"][:], ALU.add)
            dv("fre", V["fre"][:], V["den"][:], ALU.mult)
            dv("t1", V["lbi"][:], lre, ALU.mult)
            dv("t2", V["y"][:], lim, ALU.mult)
            dv("fim", V["t1"][:], V["t2"][:], ALU.subtract)
            dv("fim", V["fim"][:], V["den"][:], ALU.mult)
            k.ts("dve", V["nfim"][:], V["fim"][:], -1.0, None, ALU.mult, reads=[V["fim"]], writes=[V["nfim"]])
            if os.environ.get("S5_DUMP") == "1":
                dd = k.sbuf("s_dd", [128, 4, 256], F32)
                k.memset("dve", dd[:], 0.0, writes=[dd])
                for ii, nm in enumerate(["fre", "fim", "mag", "cb", "sb", "th", "step"]):
                    k.cp("dve", dd[:, 0, ii * 32:(ii + 1) * 32], V[nm][:], reads=[V[nm]], writes=[dd])
                k.dma("sp", dbg_out, dd[:], reads=[dd], writes=[dbgT])
                return
            rt = k.sbuf("s_rt", [128, 16, 128], F32)
            ctab = k.sbuf("s_ctab", [128, 16, 128], F32)
            stab = k.sbuf("s_stab", [128, 16, 2, 128], F32)
            cL_t = k.sbuf("s_cL", [128, 32], F32)
            sL_t = k.sbuf("s_sL", [128, 32], F32)
            nsL_t = k.sbuf("s_nsL", [128, 32], F32)
            thn = k.sbuf("s_thn", [128, 32], F32)
            k.ts("dve", thn[:], V["th"][:], 1.0 / (2 * math.pi), None, ALU.mult, reads=[V["th"]], writes=[thn])
            iot = k.sbuf("s_iot", [128, 128], F32)
            k.ts("dve", iot[:], cmf[:, 6, :], -1.0, None, ALU.add, reads=[cmf], writes=[iot])
            A = {n_: k.sbuf("s_A" + n_, [128, 128], F32) for n_ in ("y", "t1", "t2")}
            Ai = k.sbuf("s_Ai", [128, 128], I32)

            def sincos(y_in, w, cos_out, sin_out, rd):
                y, t1, t2 = A["y"][:, 0:w], A["t1"][:, 0:w], A["t2"][:, 0:w]
                allA = list(A.values())
                k.cp("dve", Ai[:, 0:w], y_in, reads=rd, writes=[Ai])
                k.cp("dve", t1, Ai[:, 0:w], reads=[Ai], writes=[A["t1"]])
                k.tt("dve", y, y_in, t1, ALU.subtract, reads=rd + allA, writes=[A["y"]])

                def wrap_():
                    k.ts("dve", t1, y, 0.5, None, ALU.is_gt, reads=allA, writes=[A["t1"]])
                    k.ts("dve", t2, y, -0.5, None, ALU.is_lt, reads=allA, writes=[A["t2"]])
                    k.tt("dve", y, y, t1, ALU.subtract, reads=allA, writes=[A["y"]])
                    k.tt("dve", y, y, t2, ALU.add, reads=allA, writes=[A["y"]])
                wrap_()
                k.act(sin_out, y, AF.Sin, reads=allA, writes=rd, scale=2 * math.pi)
                k.ts("dve", y, y, 0.25, None, ALU.add, reads=allA, writes=[A["y"]])
                wrap_()
                k.act(cos_out, y, AF.Sin, reads=allA, writes=rd, scale=2 * math.pi)

            th128 = k.sbuf("s_th128", [128, 32], F32)
            k.ts("dve", th128[:], thn[:], 128.0, None, ALU.mult, reads=[thn], writes=[th128])
            sincos(th128[:], 32, cL_t[:], sL_t[:], [th128, cL_t, sL_t])
            k.ts("dve", nsL_t[:], sL_t[:], -1.0, None, ALU.mult, reads=[sL_t], writes=[nsL_t])
            cL, sL, nsL = cL_t[:], sL_t[:], nsL_t[:]
            ck, sk, nsk = cL_t, sL_t, nsL_t
            ang = k.sbuf("s_ang", [128, 128], F32)
            Bre = k.sbuf("s_Bre", [128, 16, 16], F32)
            Bim = k.sbuf("s_Bim", [128, 16, 16], F32)
            k.dma("sp", Bre[:], I["s5_b_re"][0].rearrange("(st gl) p c -> (gl p) st c", gl=2), writes=[Bre])
            k.dma("sp", Bim[:], I["s5_b_im"][0].rearrange("(st gl) p c -> (gl p) st c", gl=2), writes=[Bim])
            s5mf = k.sbuf("s_msk", [128, 4, 128], F32)
            k.dma("sp", s5mf[:], I["s5m"], writes=[s5mf])
            drv = k.sbuf("s_drv", [128, 16, 2, 128], BF16)
            rdo = k.sbuf("s_rdo", [128, 16, 2, 128], BF16)
            bx = [k.sbuf(f"s_bx{i}", [128, 8, 16], F32) for i in range(2)]
            bx2 = [k.sbuf(f"s_bx2{i}", [128, 8, 16], F32) for i in range(2)]
            Y = [k.sbuf(f"s_Y{i}", [128, 4, 128], F32) for i in range(2)]
            gcnt = [0]

            def gen_dir(d):
                for st in range(16):
                    ds = d * 16 + st
                    k.ts("dve", rt[:, st, :], onesf, V["mag"][:, ds:ds + 1], None, ALU.mult, reads=[cmf, V["mag"]], writes=[rt])
                for st in range(16):
                    ds = d * 16 + st
                    k.ts("dve", ang[:], iot[:], thn[:, ds:ds + 1], None, ALU.mult, reads=[iot, thn], writes=[ang])
                    sincos(ang[:], 128, ctab[:, st, :], stab[:, st, 0, :], [ang, ctab, stab])
                k.ts("dve", stab[:, :, 1, :], stab[:, :, 0, :], -1.0, None, ALU.mult, reads=[stab], writes=[stab])
                for st in range(16):
                    ds = d * 16 + st
                    q = st % 4
                    for ri in range(2):
                        gcnt[0] += 1
                        b_, b2_ = bx[gcnt[0] % 2], bx2[gcnt[0] % 2]
                        B1 = (Bre if ri == 0 else Bim)[:, st, :].unsqueeze(1).to_broadcast([128, 8, 16])
                        B2 = (Bim if ri == 0 else Bre)[:, st, :].unsqueeze(1).to_broadcast([128, 8, 16])
                        f2 = V["nfim"] if ri == 0 else V["fim"]
                        k.ts("dve", b_[:], B1, V["fre"][:, ds:ds + 1], None, ALU.mult, reads=[Bre, Bim, V["fre"]], writes=[b_])
                        k.stt(b2_[:], B2, f2[:, ds:ds + 1], b_[:], ALU.mult, ALU.add, reads=[Bre, Bim, f2, b_], writes=[b2_])
                        k.tt("dve", b_[:].rearrange("p a b -> p (a b)"), b2_[:].rearrange("p a b -> p (a b)"), s5mf[:, q, :], ALU.mult,
                             reads=[b2_, s5mf], writes=[b_])
                        p_ = pf()
                        k.tr(p_, p_[:, 0:128], b_[:].rearrange("p a b -> p (a b)"), identf, reads=[b_, cmf])
                        k.cp("act", drv[:, st, ri, :], p_[:, 0:128], reads=[p_], writes=[drv])
                for oc in range(4):
                    for ri in range(2):
                        gcnt[0] += 1
                        y_ = Y[gcnt[0] % 2]
                        k.memset("dve", y_[:], 0.0, writes=[y_])
                        src = I["s5_c_re" if ri == 0 else "s5_c_im"]
                        for gi in range(8):
                            g = oc * 8 + gi
                            k.dma("sp", y_[gi * 16:(gi + 1) * 16, gi // 2, (gi % 2) * 64:(gi % 2 + 1) * 64], src[0, d, g], writes=[y_])
                        for s4 in range(4):
                            st = oc * 4 + s4
                            p_ = pf()
                            k.tr(p_, p_[:, 0:128], y_[:, s4, :], identf, reads=[y_, cmf])
                            if ri == 0:
                                k.cp("act", rdo[:, st, 0, :], p_[:, 0:128], reads=[p_], writes=[rdo])
                            else:
                                k.ts("dve", rdo[:, st, 1, :], p_[:, 0:128], -1.0, None, ALU.mult, reads=[p_], writes=[rdo])
            Wu = k.sbuf("s_Wu", [128, 8, 512], BF16)
            for kc in range(8):
                k.dma("pool", Wu[:, kc, :], I["ab_w_in"][0, kc * 128:(kc + 1) * 128, 1568:2080], writes=[Wu])
            glw = k.sbuf("s_glw", [128, 4, 512], BF16)
            k.dma("pool", glw[:], I["s5_glu_w"][0].rearrange("(kc p) n -> p kc n", p=128), writes=[glw])
            v3 = k.sbuf("s_v3", [8, 128], F32)
            k.dma("sp", v3[0:4, :], I["s5_d"][0].rearrange("(j p) -> j p", p=128), writes=[v3])
            k.dma("sp", v3[4:8, :], I["s5_glu_b"][0].rearrange("(j p) -> j p", p=128), writes=[v3])
            p_ = pf()
            k.tr(p_, p_[:, 0:8], v3[:], identf[0:8, 0:8], reads=[v3, cmf])
            dgb = k.sbuf("s_dgb", [128, 8], F32)
            k.cp("dve", dgb[:], p_[:, 0:8], reads=[p_], writes=[dgb])
            uT = [k.sbuf(f"s_uT{i}", [128, 4, 128], BF16) for i in range(2)]
            bt = [k.sbuf(f"s_bt{i}", [128, 2, 128], F32) for i in range(2)]
            t1_ = [k.sbuf(f"s_t1{i}", [128, 2, 128], F32) for i in range(2)]
            t2_ = [k.sbuf(f"s_t2{i}", [128, 2, 128], F32) for i in range(2)]
            wv = [k.sbuf(f"s_w{i}", [128, 2, 128], F32) for i in range(2)]
            o1 = [k.sbuf(f"s_o1{i}", [128, 2, 128], F32) for i in range(2)]
            o2 = [k.sbuf(f"s_o2{i}", [128, 2, 128], F32) for i in range(2)]
            sri = [k.sbuf(f"s_sri{i}", [128, 4, 2, 128], BF16) for i in range(2)]
            winit = k.sbuf("s_winit", [128, 32, 2], F32)
            wtmp = k.sbuf("s_wtmp", [128, 2], F32)
            yf32 = [k.sbuf(f"s_yf{i}", [128, 4, 128], F32) for i in range(2)]
            yin = [k.sbuf(f"s_yin{i}", [128, 4, 128], F32) for i in range(2)]
            yy = k.sbuf("s_yy", [128, 4, 128], F32)
            q1 = k.sbuf("s_q1", [128, 4, 128], F32)
            yg = k.sbuf("s_yg", [128, 4, 128], BF16)
            yo = [k.sbuf(f"s_yo{i}", [128, 4, 128], BF16) for i in range(2)]
            k.memset("dve", winit[:], 0.0, writes=[winit])
            n = 0
            for d in range(2):
                order = list(range(NT)) if d == 0 else [1, 0] + list(range(NT - 1, 1, -1))

                if os.environ.get("GLA_LIM"):
                    order = order[:int(os.environ["GLA_LIM"])]
                sl = slice(None, None, None) if d == 0 else slice(None, None, -1)
                last = 127 if d == 0 else 0
                gen_dir(d)
                for i in order:
                    hr = hreads(i)
                    n += 1
                    u_ = uT[n % 2]
                    pu = pf()
                    for c in range(4):
                        for kc in range(8):
                            k.mm(pu, pu[:, c * 128:(c + 1) * 128], Wu[:, kc, c * 128:(c + 1) * 128], hTi(kc, i),
                                 start=(kc == 0), stop=(kc == 7), reads=[Wu] + hr)
                    k.cp("act", u_[:].rearrange("p a b -> p (a b)"), pu[:, 0:512], reads=[pu], writes=[u_])
                    py = PX
                    for oc in range(4):
                        s_ = sri[(n * 4 + oc) % 2]
                        for s4 in range(4):
                            st = oc * 4 + s4
                            ds = d * 16 + st
                            m = (n * 16 + st) % 2
                            pd = pf()
                            for ri in range(2):
                                k.mm(pd, pd[:, ri * 128:(ri + 1) * 128], drv[:, st, ri, :], u_[:, oc, :], reads=[drv, u_])
                            if os.environ.get("S5_DUMP") == "4" and d == 0 and i == 0 and st == 0:
                                k.cp("dve", yy[:, 0:2, :].rearrange("p a b -> p (a b)"), pd[:, 0:256], reads=[pd], writes=[yy])
                                k.dma("sp", dbg_out[:, 0, :], yy[:, 0:2, :].rearrange("p a b -> p (a b)"), reads=[yy], writes=[dbgT])
                            P3 = pd[:, 0:256].rearrange("p (a b) -> p a b", a=2)
                            ctb = ctab[:, st, sl].unsqueeze(1).to_broadcast([128, 2, 128])
                            stb = stab[:, st, :, sl]
                            k.tt("dve", t1_[m][:], P3, ctb, ALU.mult, reads=[pd, ctab], writes=[t1_[m]])
                            k.tt("dve", t2_[m][:], P3[:, ::-1, :], stb, ALU.mult, reads=[pd, stab], writes=[t2_[m]])
                            k.tt("pool", bt[m][:], t1_[m][:], t2_[m][:], ALU.add, reads=[t1_[m], t2_[m]], writes=[bt[m]])
                            for ri in range(2):
                                k.op("dve", lambda e, m=m, ri=ri, ds=ds, st=st, sl=sl: e.tensor_tensor_scan(
                                    out=wv[m][:, ri, sl], data0=rt[:, st, :], data1=bt[m][:, ri, sl], initial=winit[:, ds, ri:ri + 1],
                                    op0=ALU.mult, op1=ALU.add), reads=[rt, bt[m], winit], writes=[wv[m]])
                            if os.environ.get("S5_DUMP") == "4" and d == 0 and i == 0 and st == 0:
                                k.dma("sp", dbg_out[:, 2, :], bt[m][:].rearrange("p a b -> p (a b)"), reads=[bt[m]], writes=[dbgT])
                                k.dma("sp", dbg_out[:, 3, :], wv[m][:].rearrange("p a b -> p (a b)"), reads=[wv[m]], writes=[dbgT])
                            k.ts("dve", wtmp[:, 0:1], wv[m][:, 0, last:last + 1], cL[:, ds:ds + 1], None, ALU.mult, reads=[wv[m], ck], writes=[wtmp])
                            k.ts("dve", wtmp[:, 1:2], wv[m][:, 1, last:last + 1], cL[:, ds:ds + 1], None, ALU.mult, reads=[wv[m], ck], writes=[wtmp])
                            k.stt(winit[:, ds, 0:1], wv[m][:, 1, last:last + 1], nsL[:, ds:ds + 1], wtmp[:, 0:1], ALU.mult, ALU.add,
                                  reads=[wv[m], nsk, wtmp], writes=[winit])
                            k.stt(winit[:, ds, 1:2], wv[m][:, 0, last:last + 1], sL[:, ds:ds + 1], wtmp[:, 1:2], ALU.mult, ALU.add,
                                  reads=[wv[m], sk, wtmp], writes=[winit])
                            k.tt("pool", o1[m][:], wv[m][:], ctb, ALU.mult, reads=[wv[m], ctab], writes=[o1[m]])
                            k.tt("pool", o2[m][:], wv[m][:, ::-1, :], stb, ALU.mult, reads=[wv[m], stab], writes=[o2[m]])
                            k.tt("pool", s_[:, s4, :, :], o1[m][:], o2[m][:], ALU.subtract, reads=[o1[m], o2[m]], writes=[s_])
                            if os.environ.get("S5_DUMP") == "4" and d == 0 and i == 0 and st == 0:
                                k.tt("pool", yy[:, 2:4, :], o1[m][:], o2[m][:], ALU.subtract, reads=[o1[m], o2[m]], writes=[yy])
                                k.dma("sp", dbg_out[:, 1, :], yy[:, 2:4, :].rearrange("p a b -> p (a b)"), reads=[yy], writes=[dbgT])
                        for s4 in range(4):
                            for ri in range(2):
                                k.mm(py, py[:, oc * 128:(oc + 1) * 128], rdo[:, oc * 4 + s4, ri, :], s_[:, s4, ri, :],
                                     start=(s4 == 0 and ri == 0), stop=(s4 == 3 and ri == 1), reads=[rdo, s_])
                    if d == 0:
                        y_ = yf32[n % 2]
                        k.cp("act", y_[:].rearrange("p a b -> p (a b)"), py[:, 0:512], reads=[py], writes=[y_])
                        k.dma("sp", yfD[:, i * 128:(i + 1) * 128].rearrange("(c p) t -> p c t", p=128), y_[:], reads=[y_], writes=[yfD.sub(i)])
                        if os.environ.get("S5_DUMP") == "3" and i < 2:
                            k.dma("sp", dbg_out[:, :, i * 128:(i + 1) * 128], y_[:], reads=[y_], writes=[dbgT])
                    else:
                        yi_ = yin[n % 2]
                        k.dma("sp", yi_[:], yfD[:, i * 128:(i + 1) * 128].rearrange("(c p) t -> p c t", p=128), reads=[yfD.sub(i)], writes=[yi_])
                        k.tt("dve", yy[:].rearrange("p a b -> p (a b)"), py[:, 0:512], yi_[:].rearrange("p a b -> p (a b)"), ALU.add,
                             reads=[py, yi_], writes=[yy])
                        for c in range(4):
                            k.stt(yy[:, c, :], u_[:, c, :], dgb[:, c:c + 1], yy[:, c, :], ALU.mult, ALU.add, reads=[u_, dgb, yy], writes=[yy])
                        if os.environ.get("S5_DUMP") == "2" and i < 2:
                            k.dma("sp", dbg_out[:, :, i * 128:(i + 1) * 128], yy[:], reads=[yy], writes=[dbgT])
                        yf = yy[:].rearrange("p a b -> p (a b)")
                        qf = q1[:].rearrange("p a b -> p (a b)")
                        k.tt("pool", qf, yf, yf, ALU.mult, reads=[yy], writes=[q1])
                        k.ts("dve", qf, qf, 0.044715, 1.0, ALU.mult, ALU.add, reads=[q1], writes=[q1])
                        k.tt("dve", qf, qf, yf, ALU.mult, reads=[q1, yy], writes=[q1])
                        k.act(qf, qf, AF.Sigmoid, reads=[q1], writes=[q1], scale=1.5957691216)
                        k.tt("dve", yf, yf, qf, ALU.mult, reads=[yy, q1], writes=[yy])
                        k.cp("pool", yg[:], yy[:], reads=[yy], writes=[yg])
                        pz = pf()
                        for c in range(4):
                            for kc in range(4):
                                k.mm(pz, pz[:, c * 128:(c + 1) * 128], glw[:, kc, c * 128:(c + 1) * 128], yg[:, kc, :],
                                     start=(kc == 0), stop=(kc == 3), reads=[glw, yg])
                        for c in range(4):
                            k.act(q1[:, c, :], pz[:, c * 128:(c + 1) * 128], AF.Sigmoid, reads=[pz, dgb], writes=[q1], bias=dgb[:, 4 + c:5 + c])
                        yo_ = yo[n % 2]
                        k.tt("dve", yo_[:].rearrange("p a b -> p (a b)"), yf, qf, ALU.mult, reads=[yy, q1], writes=[yo_])
                        k.dma("sp", mixT[512:1024, i * 128:(i + 1) * 128].rearrange("(c p) t -> p c t", p=128), yo_[:],
                              reads=[yo_], writes=[mixT.sub(("s", i))])
                if d == 0:
                    k.memset("dve", winit[:], 0.0, writes=[winit])

    def stage_outproj(l, L, wname, nk, tiles, src, mixkeys):
        with k.scope():
            Wo = k.sbuf("o_W", [128, nk, 1024], BF16)
            for kc in range(nk):
                k.dma("pool", Wo[:, kc, :], I[wname][0, kc * 128:(kc + 1) * 128, :], writes=[Wo])
            mt = [k.sbuf(f"o_m{i}", [128, nk, 128], BF16) for i in range(2)]
            xt = [k.sbuf(f"o_x{i}", [128, 1024], F32) for i in range(2)]
            for n, i in enumerate(tiles):
                s = 1 if i < 2 else 0
                m_, x_ = mt[n % 2], xt[n % 2]
                k.dma("sp", m_[:], mixT[0:nk * 128, i * 128:(i + 1) * 128].rearrange("(c p) t -> p c t", p=128),
                      reads=[mixT.sub((key, i)) for key in mixkeys], writes=[m_])
                k.dma("act", x_[:], src[i * 128:(i + 1) * 128, :], reads=[src.sub(i)], writes=[x_])
                for hf in range(2):
                    p_ = pf()
                    for kc in range(nk):
                        k.mm(p_, p_[:, 0:512], m_[:, kc, :], Wo[:, kc, hf * 512:(hf + 1) * 512], start=(kc == 0), stop=(kc == nk - 1), reads=[m_, Wo])
                    g = L["grow"][0][s]
                    k.tt("dve", p_[:, 0:512], p_[:, 0:512], g[:, hf * 512:(hf + 1) * 512], ALU.mult, reads=[p_, g], writes=[p_])
                    k.tt("dve", x_[:, hf * 512:(hf + 1) * 512], x_[:, hf * 512:(hf + 1) * 512], p_[:, 0:512], ALU.add, reads=[x_, p_], writes=[x_])
                k.dma("sp", xres[i * 128:(i + 1) * 128, :], x_[:], reads=[x_], writes=[xres.sub(i)])

    def stage_moe(l, L, tiles, final):
        with k.scope():
            lim = int(os.environ.get("MOE_E", "32"))
            G = k.sbuf("e_G", [128, NT, 32], F32)
            wr = k.sbuf("e_wr", [128, 8, 32], BF16)
            k.dma("pool", wr[:], I["moe_w_router"][l].rearrange("(kc p) n -> p kc n", p=128), writes=[wr])
            brow = k.sbuf("e_brow", [128, 32], F32)
            k.dma("sp", brow[:], I["moe_b_router"][l].partition_broadcast(128), writes=[brow])
            lg = k.sbuf("e_lg", [128, 32], F32)
            m8 = k.sbuf("e_m8", [128, 8], F32)
            nm = k.sbuf("e_nm", [128, 1], F32)
            msk = k.sbuf("e_msk", [128, 32], F32)
            ex = k.sbuf("e_ex", [128, 32], F32)
            den = k.sbuf("e_den", [128, 1], F32)
            for i in tiles:
                p_ = pf()
                for kc in range(8):
                    k.mm(p_, p_[:, 0:32], hTi(kc, i), wr[:, kc, :], start=(kc == 0), stop=(kc == 7), reads=[wr] + hreads(i))
                k.tt("dve", lg[:], p_[:, 0:32], brow[:], ALU.add, reads=[p_, brow], writes=[lg])
                k.op("dve", lambda e: e.max(out=m8[:], in_=lg[:]), reads=[lg], writes=[m8])
                k.ts("dve", msk[:], lg[:], m8[:, 3:4], None, ALU.is_ge, reads=[lg, m8], writes=[msk])
                k.ts("dve", nm[:], m8[:, 0:1], -1.0, None, ALU.mult, reads=[m8], writes=[nm])
                k.act(ex[:], lg[:], AF.Exp, reads=[lg, nm], writes=[ex], bias=nm[:, 0:1])
                k.tt("dve", ex[:], ex[:], msk[:], ALU.mult, reads=[ex, msk], writes=[ex])
                k.op("dve", lambda e: e.reduce_sum(out=den[:], in_=ex[:], axis=AX.X), reads=[ex], writes=[den])
                k.op("dve", lambda e: e.reciprocal(out=den[:], in_=den[:]), reads=[den], writes=[den])
                k.ts("dve", G[:, i, :], ex[:], den[:, 0:1], None, ALU.mult, reads=[ex, den], writes=[G])
            bst = k.sbuf("e_bst", [128, 4, 128], F32)
            k.dma("sp", bst[:], I["moe_b_gu"][l].rearrange("e (j p) -> (e j) p", p=128).rearrange("(a r) p -> r a p", r=128), writes=[bst])
            bguT = k.sbuf("e_bguT", [128, 512], F32)
            for a in range(4):
                p_ = pf()
                k.tr(p_, p_[:, 0:128], bst[:, a, :], identf, reads=[bst, cmf])
                k.cp("dve", bguT[:, a * 128:(a + 1) * 128], p_[:, 0:128], reads=[p_], writes=[bguT])
            bd = k.sbuf("e_bd", [32, 1024], F32)
            k.dma("sp", bd[:], I["moe_b_down"][l], writes=[bd])
            wgu = k.sbuf("e_wgu", [128, 8, 2048], BF16)
            wd = k.sbuf("e_wd", [128, 8, 1024], BF16)
            acc = k.sbuf("e_acc", [128, 6, 1024], F32)
            actT = k.sbuf("e_actT", [128, 8, 512], BF16)
            gc = k.sbuf("e_gc", [128, 512], F32)
            sgm = k.sbuf("e_sg", [128, 512], F32)
            lc = k.sbuf("e_lc", [128, 512], F32)
            GT = k.sbuf("e_GT", [32, 128], F32)
            xt = [k.sbuf(f"e_x{i}", [128, 1024], F32) for i in range(1)]
            junk = k.sbuf("e_junk", [128, 1024], BF16)
            ssq = k.sbuf("e_ssq", [128, 1], F32)
            fng = k.sbuf("e_fng", [128, 1024], F32)
            if final:
                k.dma("sp", fng[:], I["final_norm_g"].partition_broadcast(128), writes=[fng])
            blocks = [tiles[a:a + 6] for a in range(0, len(tiles), 6)]
            for blk in blocks:
                for e in range(lim):
                    for kc in range(8):
                        k.dma("pool", wgu[:, kc, :], I["moe_w_gu"][l, e, kc * 128:(kc + 1) * 128, :], writes=[wgu])
                    for kc in range(8):
                        k.dma("pool", wd[:, kc, :], I["moe_w_down"][l, e, kc * 128:(kc + 1) * 128, :], writes=[wd])
                    for sb0 in range(0, len(blk), 4):
                        sb = blk[sb0:sb0 + 4]
                        N = len(sb) * 128
                        t0 = sb[0]
                        hr = hreads(t0, len(sb))
                        for c in range(8):
                            pg, pl = pf(), pf()
                            for kc in range(8):
                                k.mm(pg, pg[:, 0:N], wgu[:, kc, c * 128:(c + 1) * 128], hT[:, kc, t0 * 128:t0 * 128 + N],
                                     start=(kc == 0), stop=(kc == 7), reads=[wgu] + hr)
                            for kc in range(8):
                                k.mm(pl, pl[:, 0:N], wgu[:, kc, 1024 + c * 128:1024 + (c + 1) * 128], hT[:, kc, t0 * 128:t0 * 128 + N],
                                     start=(kc == 0), stop=(kc == 7), reads=[wgu] + hr)
                            k.ts("dve", gc[:, 0:N], pg[:, 0:N], bguT[:, e * 16 + c:e * 16 + c + 1], 7.0, ALU.add, ALU.min, reads=[pg, bguT], writes=[gc])
                            k.act(sgm[:, 0:N], gc[:, 0:N], AF.Sigmoid, reads=[gc], writes=[sgm], scale=1.702)
                            k.ts("dve", lc[:, 0:N], pl[:, 0:N], bguT[:, e * 16 + 8 + c:e * 16 + 8 + c + 1], 7.0, ALU.add, ALU.min, reads=[pl, bguT], writes=[lc])
                            k.ts("dve", lc[:, 0:N], lc[:, 0:N], -7.0, 1.0, ALU.max, ALU.add, reads=[lc], writes=[lc])
                            k.tt("pool", gc[:, 0:N], gc[:, 0:N], sgm[:, 0:N], ALU.mult, reads=[gc, sgm], writes=[gc])
                            k.tt("dve", actT[:, c, 0:N], gc[:, 0:N], lc[:, 0:N], ALU.mult, reads=[gc, lc], writes=[actT])
                        for ti, i in enumerate(sb):
                            bi = blk.index(i)
                            for hf in range(2):
                                pd_ = pf()
                                for c in range(8):
                                    k.mm(pd_, pd_[:, 0:512], actT[:, c, ti * 128:(ti + 1) * 128], wd[:, c, hf * 512:(hf + 1) * 512],
                                         start=(c == 0), stop=(c == 7), reads=[actT, wd])
                                a_ = acc[:, bi, hf * 512:(hf + 1) * 512]
                                if e == 0:
                                    k.ts("dve", a_, pd_[:, 0:512], G[:, i, e:e + 1], None, ALU.mult, reads=[pd_, G], writes=[acc])
                                else:
                                    k.stt(a_, pd_[:, 0:512], G[:, i, e:e + 1], a_, ALU.mult, ALU.add, reads=[pd_, G, acc], writes=[acc])
                for bi, i in enumerate(blk):
                    s = 1 if i < 2 else 0
                    p_ = pf()
                    k.tr(p_, p_[0:32, 0:128], G[:, i, :], identf, reads=[G, cmf])
                    k.cp("dve", GT[:], p_[0:32, 0:128], reads=[p_], writes=[GT])
                    x_ = xt[0]
                    k.dma("sp", x_[:], xres[i * 128:(i + 1) * 128, :], reads=[xres.sub(i)], writes=[x_])
                    g = L["grow"][1][s]
                    for hf in range(2):
                        p2 = pf()
                        k.mm(p2, p2[:, 0:512], GT[:], bd[:, hf * 512:(hf + 1) * 512], reads=[GT, bd])
                        a_ = acc[:, bi, hf * 512:(hf + 1) * 512]
                        k.tt("dve", a_, a_, p2[:, 0:512], ALU.add, reads=[acc, p2], writes=[acc])
                        k.tt("dve", a_, a_, g[:, hf * 512:(hf + 1) * 512], ALU.mult, reads=[acc, g], writes=[acc])
                        k.tt("dve", x_[:, hf * 512:(hf + 1) * 512], x_[:, hf * 512:(hf + 1) * 512], a_, ALU.add, reads=[x_, acc], writes=[x_])
                    if not final:
                        k.dma("sp", xres[i * 128:(i + 1) * 128, :], x_[:], reads=[x_], writes=[xres.sub(i)])
                    else:
                        k.act(junk[:], x_[:], AF.Square, reads=[x_], writes=[junk, ssq], accum_out=ssq[:])
                        k.act(ssq[:], ssq[:], AF.Sqrt, reads=[ssq], writes=[ssq], scale=1.0 / 1024, bias=EPS)
                        k.op("dve", lambda e: e.reciprocal(out=ssq[:], in_=ssq[:]), reads=[ssq], writes=[ssq])
                        k.stt(x_[:], x_[:], ssq[:, 0:1], fng[:], ALU.mult, ALU.mult, reads=[x_, ssq, fng], writes=[x_])
                        k.dma("sp", yout[(i - 2) * 128:(i - 1) * 128, :], x_[:], reads=[x_], writes=[youtT])

    def stage_ret():
        lim = os.environ.get("RET_LIM")
        with k.scope():
            gm1 = k.sbuf("r_lg", [128, 8], F32)
            k.dma("sp", gm1[:], I["ret_decay_logit"][0].rearrange("d h -> (d h)").partition_broadcast(128), writes=[gm1])
            lgam = k.sbuf("r_lgam", [128, 8], F32)
            nlg = k.sbuf("r_nlg", [128, 8], F32)
            k.act(lgam[:], gm1[:], AF.Exp, reads=[gm1], writes=[lgam], scale=-1.0)
            k.act(lgam[:], lgam[:], AF.Ln, reads=[lgam], writes=[lgam], bias=1.0)
            k.cp("dve", nlg[:], lgam[:], reads=[lgam], writes=[nlg])
            k.ts("dve", lgam[:], lgam[:], -1.0, None, ALU.mult, reads=[lgam], writes=[lgam])
            g128 = k.sbuf("r_g128", [128, 8], F32)
            k.act(g128[:], lgam[:], AF.Exp, reads=[lgam], writes=[g128], scale=128.0)
            ngrow = k.sbuf("r_ng", [128, 512], F32)
            Wq = k.sbuf("r_Wq", [128, 8, 256], BF16)
            Wk = k.sbuf("r_Wk", [128, 8, 256], BF16)
            Wqs = k.sbuf("r_Wqs", [128, 8, 256], BF16)
            Wks = k.sbuf("r_Wks", [128, 8, 256], BF16)
            Wv = k.sbuf("r_Wv", [128, 8, 512], BF16)
            Wg = k.sbuf("r_Wg", [128, 8, 512], BF16)
            Dq = k.sbuf("r_Dq", [128, 128], F32)
            Dk = k.sbuf("r_Dk", [128, 128], F32)
            S32 = k.sbuf("r_S32", [128, 2, 512], F32)
            Sbf = k.sbuf("r_Sbf", [128, 2, 512], BF16)
            rp = [k.sbuf(f"r_rp{i}", [128, 2, 2, 128], F32) for i in range(2)]
            qd = k.sbuf("r_qd", [128, 2, 128], BF16)
            ki = k.sbuf("r_ki", [128, 2, 128], BF16)
            kt = k.sbuf("r_kt", [128, 256], BF16)
            tq = k.sbuf("r_tq", [128, 4, 128], F32)
            tq2 = k.sbuf("r_tq2", [128, 4, 128], F32)
            vb = k.sbuf("r_vb", [128, 512], BF16)
            ST = k.sbuf("r_ST", [128, 128], BF16)
            o32 = [k.sbuf(f"r_o{i}", [128, 512], F32) for i in range(2)]
            oin = [k.sbuf(f"r_oi{i}", [128, 512], F32) for i in range(2)]
            sg = k.sbuf("r_sg", [128, 512], F32)
            st6 = k.sbuf("r_st6", [128, 6], F32)
            mv = k.sbuf("r_mv", [128, 2], F32)
            fin = k.sbuf("r_fin", [128, 512], BF16)
            finT = [k.sbuf(f"r_fT{i}", [128, 4, 128], BF16) for i in range(2)]
            wv_ = I["ret_w_in"][0]
            cnt = 0
            for h in range(4):
                for kc in range(8):
                    r0 = kc * 128
                    k.dma("pool", Wq[:, kc, :], wv_[r0:r0 + 128, h * 256:(h + 1) * 256], writes=[Wq])
                    k.dma("pool", Wk[:, kc, :], wv_[r0:r0 + 128, 1024 + h * 256:1024 + (h + 1) * 256], writes=[Wk])
                    for c in range(2):
                        for hf in range(2):
                            k.dma("pool", Wqs[:, kc, c * 128 + hf * 64:c * 128 + (hf + 1) * 64],
                                  wv_[r0:r0 + 128, h * 256 + c * 128 + (1 - hf) * 64:h * 256 + c * 128 + (2 - hf) * 64], writes=[Wqs])
                            k.dma("pool", Wks[:, kc, c * 128 + hf * 64:c * 128 + (hf + 1) * 64],
                                  wv_[r0:r0 + 128, 1024 + h * 256 + c * 128 + (1 - hf) * 64:1024 + h * 256 + c * 128 + (2 - hf) * 64], writes=[Wks])
                    k.dma("pool", Wv[:, kc, :], wv_[r0:r0 + 128, 2048 + h * 512:2048 + (h + 1) * 512], writes=[Wv])
                    k.dma("pool", Wg[:, kc, :], wv_[r0:r0 + 128, 4096 + h * 512:4096 + (h + 1) * 512], writes=[Wg])
                k.dma("sp", ngrow[:], I["ret_norm_g"][0, h * 512:(h + 1) * 512].partition_broadcast(128), writes=[ngrow])
                for d in range(2):
                    order = list(range(NT)) if d == 0 else [1, 0] + list(range(NT - 1, 1, -1))
                    if lim:
                        order = order[:int(lim)]
                    col = d * 4 + h
                    pos = cmf[:, 6, :] if d == 0 else cmf[:, 7, :]
                    k.act(Dq[:], pos, AF.Exp, reads=[cmf, lgam], writes=[Dq], scale=lgam[:, col:col + 1])
                    k.act(Dk[:], pos, AF.Exp, reads=[cmf, nlg], writes=[Dk], scale=nlg[:, col:col + 1])
                    k.memset("dve", S32[:], 0.0, writes=[S32])
                    k.memset("dve", Sbf[:], 0.0, writes=[Sbf])
                    mask = cmb[:, 1 + d, :]
                    for i in order:
                        hr = hreads(i)
                        cnt += 1
                        lat = i >= 2
                        pq = pf()
                        for gi, Wm in enumerate((Wq, Wk)):
                            for c in range(2):
                                for kc in range(8):
                                    k.mm(pq, pq[:, (gi * 2 + c) * 128:(gi * 2 + c + 1) * 128], Wm[:, kc, c * 128:(c + 1) * 128], hTi(kc, i),
                                         start=(kc == 0), stop=(kc == 7), reads=[Wm] + hr)
                        if lat:
                            ps_ = pf()
                            for gi, Wm in enumerate((Wqs, Wks)):
                                for c in range(2):
                                    for kc in range(8):
                                        k.mm(ps_, ps_[:, (gi * 2 + c) * 128:(gi * 2 + c + 1) * 128], Wm[:, kc, c * 128:(c + 1) * 128], hTi(kc, i),
                                             start=(kc == 0), stop=(kc == 7), reads=[Wm] + hr)
                            r_ = rp[cnt % 2]
                            k.dma("sp", r_[:], I["rope"][:, :, :, (i - 2) * 128:(i - 1) * 128].rearrange("ty cs p t -> p ty cs t"), writes=[r_])
                            for gi in range(2):
                                a_ = tq[:, gi * 2:gi * 2 + 2, :]
                                k.tt("dve", a_, pq[:, gi * 256:(gi + 1) * 256].rearrange("p (a b) -> p a b", a=2), r_[:, :, 0, :], ALU.mult, reads=[pq, r_], writes=[tq])
                                b_ = tq2[:, gi * 2:gi * 2 + 2, :]
                                k.tt("dve", b_, ps_[:, gi * 256:(gi + 1) * 256].rearrange("p (a b) -> p a b", a=2), r_[:, :, 1, :], ALU.mult, reads=[ps_, r_], writes=[tq2])
                            k.tt("pool", tq[:], tq[:], tq2[:], ALU.add, reads=[tq, tq2], writes=[tq])
                        else:
                            k.cp("act", tq[:].rearrange("p a b -> p (a b)"), pq[:, 0:512], reads=[pq], writes=[tq])
                        for c in range(2):
                            k.tt("dve", qd[:, c, :], tq[:, c, :], Dq[:], ALU.mult, reads=[tq, Dq], writes=[qd])
                            k.stt(ki[:, c, :], tq[:, 2 + c, :], 0.0625, Dk[:], ALU.mult, ALU.mult, reads=[tq, Dk], writes=[ki])
                        p_ = pb()
                        for c in range(2):
                            k.tr(p_, p_[:, c * 128:(c + 1) * 128], ki[:, c, :], identb, reads=[ki, cmb])
                        k.cp("act", kt[:], p_[:, 0:256], reads=[p_], writes=[kt])
                        pv = pf()
                        for kc in range(8):
                            k.mm(pv, pv[:, 0:512], hTi(kc, i), Wv[:, kc, :], start=(kc == 0), stop=(kc == 7), reads=[Wv] + hr)
                        k.cp("act", vb[:], pv[:, 0:512], reads=[pv], writes=[vb])
                        if lat:
                            pss = pf()
                            for c in range(2):
                                k.mm(pss, pss[:, 0:128], ki[:, c, :], qd[:, c, :], start=(c == 0), stop=(c == 1), reads=[ki, qd])
                            k.tt("dve", ST[:], pss[:, 0:128], mask, ALU.mult, reads=[pss, cmb], writes=[ST])
                            po = pf()
                            for c in range(2):
                                k.mm(po, po[:, 0:512], qd[:, c, :], Sbf[:, c, :], start=(c == 0), stop=False, reads=[qd, Sbf])
                            k.mm(po, po[:, 0:512], ST[:], vb[:], start=False, stop=True, reads=[ST, vb])
                        for c in range(2):
                            pst = pf()
                            k.mm(pst, pst[:, 0:512], kt[:, c * 128:(c + 1) * 128], vb[:], reads=[kt, vb])
                            k.tt("dve", S32[:, c, :], S32[:, c, :], pst[:, 0:512], ALU.add, reads=[S32, pst], writes=[S32])
                            k.ts("dve", S32[:, c, :], S32[:, c, :], g128[:, col:col + 1], None, ALU.mult, reads=[S32, g128], writes=[S32])
                            k.cp("pool", Sbf[:, c, :], S32[:, c, :], reads=[S32], writes=[Sbf])
                        if not lat:
                            continue
                        o_ = o32[cnt % 2]
                        if d == 0:
                            k.cp("act", o_[:], po[:, 0:512], reads=[po], writes=[o_])
                            k.dma("sp", ofD[i * 128:(i + 1) * 128, h * 512:(h + 1) * 512], o_[:], reads=[o_], writes=[ofD.sub((h, i))])
                        else:
                            oi = oin[cnt % 2]
                            k.dma("sp", oi[:], ofD[i * 128:(i + 1) * 128, h * 512:(h + 1) * 512], reads=[ofD.sub((h, i))], writes=[oi])
                            k.tt("dve", o_[:], po[:, 0:512], oi[:], ALU.add, reads=[po, oi], writes=[o_])
                            k.op("dve", lambda e, o_=o_: e.bn_stats(out=st6[:], in_=o_[:]), reads=[o_], writes=[st6])
                            k.op("dve", lambda e: e.bn_aggr(out=mv[:], in_=st6[:]), reads=[st6], writes=[mv])
                            k.act(mv[:, 1:2], mv[:, 1:2], AF.Sqrt, reads=[mv], writes=[mv], bias=EPS)
                            k.op("dve", lambda e: e.reciprocal(out=mv[:, 1:2], in_=mv[:, 1:2]), reads=[mv], writes=[mv])
                            k.ts("dve", o_[:], o_[:], mv[:, 0:1], mv[:, 1:2], ALU.subtract, ALU.mult, reads=[o_, mv], writes=[o_])
                            pg = pf()
                            for kc in range(8):
                                k.mm(pg, pg[:, 0:512], hTi(kc, i), Wg[:, kc, :], start=(kc == 0), stop=(kc == 7), reads=[Wg] + hr)
                            k.act(sg[:], pg[:, 0:512], AF.Silu, reads=[pg], writes=[sg])
                            k.tt("dve", o_[:], o_[:], ngrow[:], ALU.mult, reads=[o_, ngrow], writes=[o_])
                            k.tt("dve", fin[:], o_[:], sg[:], ALU.mult, reads=[o_, sg], writes=[fin])
                            p_ = pb()
                            for c in range(4):
                                k.tr(p_, p_[:, c * 128:(c + 1) * 128], fin[:, c * 128:(c + 1) * 128], identb, reads=[fin, cmb])
                            ft = finT[cnt % 2]
                            k.cp("act", ft[:].rearrange("p a b -> p (a b)"), p_[:, 0:512], reads=[p_], writes=[ft])
                            k.dma("sp", mixT[h * 512:(h + 1) * 512, i * 128:(i + 1) * 128].rearrange("(c p) t -> p c t", p=128), ft[:],
                                  reads=[ft], writes=[mixT.sub((h, i))])

    def dump_rows(src, r0, nrows, ncols):
        with k.scope():
            t_ = k.sbuf("dump", [128, ncols], F32)
            for a in range(nrows // 128):
                k.dma("sp", t_[:], src[r0 + a * 128:r0 + (a + 1) * 128, 0:ncols], reads=[src] + list(src.subs.values()), writes=[t_])
                k.dma("sp", dbg_out[a * 128:(a + 1) * 128, 0:ncols], t_[:], reads=[t_], writes=[dbgT])

    L0 = {}
    with k.scope():
        L = {"vecT": k.sbuf("L_vecT", [128, 64], F32), "modF": k.sbuf("L_modF", [128, 48, 2], F32),
             "grow": [[k.sbuf(f"L_g{w}{s}", [128, 1024], F32) for s in range(2)] for w in range(2)],
             "mulA": [k.sbuf(f"L_mulA{w}", [128, 8, 2], F32) for w in range(2)]}
        stage_mod(0, L)
        pass_a(L, 0, list(range(NT)), xin_T)
        if upto >= 1:
            stage_gla()
        if upto >= 2:
            stage_s5()
        if upto >= 3:
            lim_ = os.environ.get("GLA_LIM")
            stage_outproj(0, L, "ab_w_out", 8, list(range(NT)) if not lim_ else [0, 1], xin_T, ["g", "s"])
        if upto >= 4:
            mt_ = list(range(NT)) if not os.environ.get("MOE_T") else list(range(int(os.environ["MOE_T"])))
            pass_a(L, 1, mt_, xres)
            stage_moe(0, L, mt_, False)
        if dbg is not None:
            if upto == 0:
                with k.scope():
                    t_ = k.sbuf("dump", [128, 8, 512], F32)
                    k.cp("dve", t_[:], hT[:, :, 0:512], reads=[hT] + list(hT.subs.values()), writes=[t_])
                    k.dma("sp", dbg_out, t_[:], reads=[t_], writes=[dbgT])
            elif upto == 2 and os.environ.get("S5_DUMP"):
                pass
            elif upto in (1, 2):
                r0 = 0 if upto == 1 else 512

                dc0 = int(os.environ.get("DUMP_C0", "0"))
                with k.scope():
                    t_ = k.sbuf("dump", [128, 4, 256], BF16)
                    t2 = k.sbuf("dump2", [128, 4, 256], F32)
                    k.dma("sp", t_[:], mixT[r0:r0 + 512, dc0:dc0 + 256].rearrange("(c p) t -> p c t", p=128), reads=list(mixT.subs.values()), writes=[t_])
                    k.cp("dve", t2[:], t_[:], reads=[t_], writes=[t2])
                    k.dma("sp", dbg_out, t2[:], reads=[t2], writes=[dbgT])
            elif upto in (3, 4):
                dump_rows(xres, 0, 256 if os.environ.get("GLA_LIM") else 512, 1024)
    if upto >= 5:
        lat_t = list(range(2, NT))
        if os.environ.get("RET_LIM"):
            lat_t = [t_ for t_ in lat_t if t_ >= NT - int(os.environ["RET_LIM"]) + 2 and t_ < 2 + int(os.environ["RET_LIM"]) - 2] or [2, 3]
        with k.scope():
            L = {"vecT": k.sbuf("L_vecT", [128, 64], F32), "modF": k.sbuf("L_modF", [128, 48, 2], F32),
                 "grow": [[k.sbuf(f"L_g{w}{s}", [128, 1024], F32) for s in range(2)] for w in range(2)],
                 "mulA": [k.sbuf(f"L_mulA{w}", [128, 8, 2], F32) for w in range(2)]}
            stage_mod(1, L)
            pass_a(L, 0, list(range(NT)), xres)
            stage_ret()
            stage_outproj(1, L, "ret_w_out", 16, lat_t, xres, [0, 1, 2, 3])
            pass_a(L, 1, lat_t, xres)
            stage_moe(1, L, lat_t, True)
    k.finish()
    nc.used_inputs = list(I.keys())
    return nc


def prep_inputs(inputs, cores=range(8), used=None):
    consts = make_consts()
    maps = []
    for b in cores:
        m = {}
        m["xin"] = np.ascontiguousarray(np.concatenate([inputs["ctx"][b], inputs["x"][b]], axis=0), dtype=np.float32)
        cv = np.stack([np.asarray(inputs["c"][b], np.float32), np.asarray(inputs["c_ctx"], np.float32)], 0)
        m["cvec"] = np.ascontiguousarray(cv.reshape(2, 8, 128).transpose(2, 0, 1).reshape(128, 16))
        m.update(consts)
        for name, _ in PARAMS:
            m[name] = np.ascontiguousarray(inputs[name], dtype=np.float32)
        if used is not None:
            m = {k_: v_ for k_, v_ in m.items() if k_ in used}
        maps.append(m)
    return maps


def kernel(**inputs):
    nc = build()
    maps = prep_inputs(inputs, used=nc.used_inputs)
    res = run_bass_kernel_spmd(nc, maps, core_ids=list(range(8)))
    return np.stack([r["yout"] for r in res.results], 0).astype(np.float32)
```

```python
import contextlib
import math
import numpy as np
import concourse.bass as bass
import concourse.mybir as mybir
from concourse.bass_utils import run_bass_kernel_spmd

F32 = mybir.dt.float32
BF16 = mybir.dt.bfloat16
I32 = mybir.dt.int32
AF = mybir.ActivationFunctionType
ALU = mybir.AluOpType
AX = mybir.AxisListType

NT = 34
T = NT * 128
EPS = 1e-6


class Buf:
    __slots__ = ("name", "w", "r")

    def __init__(self, name):
        self.name = name
        self.w = None
        self.r = {}


class TT:
    def __init__(self, t, name):
        self.t = t
        self.name = name
        self.b = Buf(name)
        self.subs = {}

    def sub(self, key):
        if key not in self.subs:
            self.subs[key] = Buf(f"{self.name}[{key}]")
        return self.subs[key]

    def __getitem__(self, idx):
        return self.t[idx]


class FW:
    COMPUTE = ("pe", "dve", "act", "pool")
    NDMA = 8

    def __init__(self, nc):
        self.nc = nc
        self.es = contextlib.ExitStack()
        self.scopes = [self.es]
        self.prog = {k: [] for k in ("pe", "dve", "act", "pool", "sp")}
        self.sems = {}
        self.cnt = {}
        for k in self.COMPUTE:
            self.sems[k] = self.es.enter_context(nc.semaphore(f"s_{k}"))
            self.cnt[k] = 0
        self.dma_ring = {}
        for q in ("sp", "act", "pool"):
            ring = []
            for i in range(self.NDMA):
                key = f"d_{q}{i}"
                self.sems[key] = self.es.enter_context(nc.semaphore(key))
                self.cnt[key] = 0
                ring.append(key)
            self.dma_ring[q] = [ring, 0]
        self.seen = {k: {} for k in self.prog}
        self.n_inst = 0
        self.block = self.es.enter_context(nc.Block())
        self._uid = 0

    def sbuf(self, name, shape, dtype):
        self._uid += 1
        t = self.scopes[-1].enter_context(self.nc.sbuf_tensor(f"{name}_{self._uid}", list(shape), dtype))
        return TT(t, name)

    def psum(self, name, shape, dtype=F32):
        t = self.scopes[-1].enter_context(self.nc.psum_tensor(name, list(shape), dtype))
        return TT(t, name)

    def dram(self, name, shape, dtype):
        t = self.nc.dram_tensor(name, list(shape), dtype, kind="Internal")
        return TT(t.ap(), name)

    @contextlib.contextmanager
    def scope(self):
        st = contextlib.ExitStack()
        self.scopes.append(st)
        try:
            yield
        finally:
            self.barrier()
            self.flush()
            self.scopes.pop()
            st.close()

    @staticmethod
    def _bufs(xs):
        out = []
        for x in xs:
            if x is None:
                continue
            out.append(x.b if isinstance(x, TT) else x)
        return out

    def _deps(self, reads, writes):
        deps = {}

        def add(d):
            if d is None:
                return
            k, v = d
            if deps.get(k, 0) < v:
                deps[k] = v
        for b in reads:
            add(b.w)
        for b in writes:
            add(b.w)
            for k, v in b.r.items():
                add((k, v))
        return deps

    def _waits(self, eng, deps, own_key):
        waits = []
        seen = self.seen[eng]
        for k, v in deps.items():
            if k == "pe" and own_key == "pe":
                continue
            if seen.get(k, 0) >= v:
                continue
            seen[k] = v
            waits.append((self.sems[k], v))
        return waits

    def _mark(self, reads, writes, key, val):
        for b in reads:
            b.r[key] = val
        for b in writes:
            b.w = (key, val)
            b.r = {}

    def op(self, eng, fn, reads=(), writes=()):
        reads = self._bufs(reads)
        writes = self._bufs(writes)
        deps = self._deps(reads, writes)
        waits = self._waits(eng, deps, eng)
        self.cnt[eng] += 1
        val = self.cnt[eng]
        self.prog[eng].append((waits, fn, (self.sems[eng], 1)))
        self._mark(reads, writes, eng, val)
        self.n_inst += 1

    def dma(self, q, out, in_, reads=(), writes=(), **kw):
        reads = self._bufs(reads)
        writes = self._bufs(writes)
        ring, n = self.dma_ring[q]
        key = ring[n % len(ring)]
        self.dma_ring[q][1] = n + 1
        deps = self._deps(reads, writes)
        if self.cnt[key] > 0:
            deps[key] = max(deps.get(key, 0), self.cnt[key])
        waits = self._waits(q, deps, None)
        self.cnt[key] += 16
        val = self.cnt[key]
        self.prog[q].append((waits, lambda e: e.dma_start(out=out, in_=in_, **kw), (self.sems[key], 16)))
        self._mark(reads, writes, key, val)
        self.n_inst += 1

    def barrier(self):
        for eng in self.prog:
            waits = []
            for k, v in self.cnt.items():
                if v > 0 and self.seen[eng].get(k, 0) < v and not (k == eng):
                    waits.append((self.sems[k], v))
                    self.seen[eng][k] = v
            if waits:
                self.prog[eng].append((waits, None, None))

    def flush(self):
        fw = self
        names = {"sp": "sync", "pe": "tensor", "dve": "vector", "act": "scalar", "pool": "gpsimd"}
        for engk, items in self.prog.items():
            if not items:
                continue

            def body(e, items=items):
                for waits, fn, inc in items:
                    for s, v in waits:
                        e.wait_ge(s, v)
                    if fn is not None:
                        ins = fn(e)
                        ins.then_inc(inc[0], inc[1])
            getattr(self.block, names[engk])(body)
            self.prog[engk] = []

    def finish(self):
        self.barrier()
        self.flush()
        self.es.close()

    def mm(self, pst, out, lhsT, rhs, start=True, stop=True, reads=()):
        self.op("pe", lambda e: e.matmul(out, lhsT, rhs, start=start, stop=stop), reads=reads, writes=[pst])

    def tr(self, pst, out, in_, ident, reads=()):
        self.op("pe", lambda e: e.transpose(out, in_, ident), reads=reads, writes=[pst])

    def act(self, out, in_, func, reads, writes, **kw):
        self.op("act", lambda e: e.activation(out=out, in_=in_, func=func, **kw), reads=reads, writes=writes)

    def ts(self, eng, out, in0, s1, s2, op0, op1=None, reads=(), writes=(), **kw):
        if op1 is None:
            self.op(eng, lambda e: e.tensor_scalar(out=out, in0=in0, scalar1=s1, scalar2=s2, op0=op0, **kw), reads=reads, writes=writes)
        else:
            self.op(eng, lambda e: e.tensor_scalar(out=out, in0=in0, scalar1=s1, scalar2=s2, op0=op0, op1=op1, **kw), reads=reads, writes=writes)

    def stt(self, out, in0, scalar, in1, op0, op1, reads=(), writes=(), **kw):
        self.op("dve", lambda e: e.scalar_tensor_tensor(out=out, in0=in0, scalar=scalar, in1=in1, op0=op0, op1=op1, **kw), reads=reads, writes=writes)

    def tt(self, eng, out, in0, in1, op, reads=(), writes=()):
        self.op(eng, lambda e: e.tensor_tensor(out=out, in0=in0, in1=in1, op=op), reads=reads, writes=writes)

    def cp(self, eng, out, in_, reads=(), writes=()):
        if eng == "act":
            self.op("act", lambda e: e.activation(out=out, in_=in_, func=AF.Identity), reads=reads, writes=writes)
        else:
            self.op(eng, lambda e: e.tensor_copy(out=out, in_=in_), reads=reads, writes=writes)

    def memset(self, eng, ap, val, writes=()):
        self.op(eng, lambda e: e.memset(ap, val), writes=writes)


PARAMS = [
    ("ada_w", [2, 1024, 6144]), ("ada_b", [2, 6144]), ("norm1_g", [2, 1024]), ("norm2_g", [2, 1024]),
    ("ab_w_in", [1, 1024, 2080]), ("ab_w_out", [1, 1024, 1024]), ("gla_wa", [1, 2, 16, 256]), ("gla_ba", [1, 2, 256]),
    ("gla_norm_g", [1, 128]), ("s5_lam_re", [1, 2, 32, 64]), ("s5_lam_im", [1, 2, 32, 64]), ("s5_log_step", [1, 2, 32]),
    ("s5_b_re", [1, 32, 64, 16]), ("s5_b_im", [1, 32, 64, 16]), ("s5_c_re", [1, 2, 32, 16, 64]), ("s5_c_im", [1, 2, 32, 16, 64]),
    ("s5_d", [1, 512]), ("s5_glu_w", [1, 512, 512]), ("s5_glu_b", [1, 512]),
    ("ret_w_in", [1, 1024, 6144]), ("ret_w_out", [1, 2048, 1024]), ("ret_decay_logit", [1, 2, 4]), ("ret_norm_g", [1, 2048]),
    ("moe_w_router", [2, 1024, 32]), ("moe_b_router", [2, 32]), ("moe_w_gu", [2, 32, 1024, 2048]), ("moe_b_gu", [2, 32, 2048]),
    ("moe_w_down", [2, 32, 1024, 1024]), ("moe_b_down", [2, 32, 1024]), ("final_norm_g", [1024]),
]


def make_consts():
    j = np.arange(128)[:, None]
    t = np.arange(128)[None, :]
    cm = np.zeros((128, 8, 128), np.float32)
    cm[:, 0] = (j == t)
    cm[:, 1] = (j <= t)
    cm[:, 2] = (j >= t)
    cm[:, 3] = (j > t)
    cm[:, 4] = (j < t)
    cm[:, 5] = 1.0
    cm[:, 6] = t + 1.0
    cm[:, 7] = 128.0 - t
    s5m = np.zeros((128, 4, 128), np.float32)
    for q in range(4):
        s5m[:, q] = ((t // 16) == (2 * q + (j >= 64)))
    n_freq = 64
    inv_freq = (10000.0 ** (-np.arange(n_freq, dtype=np.float32) / n_freq)).astype(np.float32)
    pos_row = (np.arange(4096) // 64).astype(np.float32)
    pos_col = (np.arange(4096) % 64).astype(np.float32)
    rope = np.zeros((2, 2, 128, 4096), np.float32)
    for ty, pos in enumerate((pos_row, pos_col)):
        ang = (pos[None, :] * inv_freq[:, None]).astype(np.float32)
        c, s = np.cos(ang), np.sin(ang)
        rope[ty, 0, :64] = c
        rope[ty, 0, 64:] = c
        rope[ty, 1, :64] = -s
        rope[ty, 1, 64:] = s
    gm = np.zeros((128, 258), np.float32)
    gm[:64, 0] = 1.0
    gm[64:, 1] = 1.0
    gm[:, 2:] = ((np.arange(128)[:, None] // 64) == (np.arange(256)[None, :] // 128))
    return {"cm": cm, "s5m": s5m, "rope": rope, "gm": gm}


def build(upto=99, dbg=None):
    import os
    GSTEP = int(os.environ.get('GLA_STEP', '99'))
    nc = bass.Bass("TRN2", target_bir_lowering=False)

    def din(name, shape, dt=F32):
        return nc.dram_tensor(name, list(shape), dt, kind="ExternalInput").ap()

    shapes = {"xin": [T, 1024], "cvec": [128, 16], "cm": [128, 8, 128], "s5m": [128, 4, 128], "rope": [2, 2, 128, 4096], "gm": [128, 258]}
    shapes.update({n_: s_ for n_, s_ in PARAMS})

    class LazyIn(dict):
        def __missing__(self, name):
            self[name] = din(name, shapes[name])
            return self[name]
    I = LazyIn()
    yout = nc.dram_tensor("yout", [4096, 1024], F32, kind="ExternalOutput").ap()
    dbg_out = None
    if dbg is not None:
        dbg_out = nc.dram_tensor("dbg", list(dbg), F32, kind="ExternalOutput").ap()

    k = FW(nc)
    xin_T = TT(I["xin"], "xin")
    xres = k.dram("xres", [T, 1024], F32)
    ofD = k.dram("ofD", [T, 2048], F32)
    yfD = k.dram("yfD", [512, T], F32)
    mixT = k.dram("mixT", [2048, T], BF16)
    dbgT = TT(dbg_out, "dbg") if dbg_out is not None else None
    youtT = TT(yout, "yout")

    cmf = k.sbuf("cmf", [128, 8, 128], F32)
    k.dma("sp", cmf[:], I["cm"], writes=[cmf])
    cmb = k.sbuf("cmb", [128, 8, 128], BF16)
    k.cp("dve", cmb[:], cmf[:], reads=[cmf], writes=[cmb])
    identf, identb = cmf[:, 0, :], cmb[:, 0, :]
    onesf, onesb = cmf[:, 5, :], cmb[:, 5, :]
    PF = [k.psum(f"pf{i}", [128, 512], F32) for i in range(5)]
    PX = k.psum("px", [128, 512], F32)
    PB = [k.psum(f"pb{i}", [128, 1024], BF16) for i in range(2)]
    pcnt = [0, 0]

    def pf():
        pcnt[0] += 1
        return PF[pcnt[0] % 5]

    def pb():
        pcnt[1] += 1
        return PB[pcnt[1] % 2]

    H = {"hT": None}

    def hTi(kc, i, n=1):
        return H["hT"][:, kc, i * 128:(i + n) * 128]

    def stage_mod(l, L):
        with k.scope():
            sc = k.sbuf("m_sc", [128, 16], F32)
            k.dma("sp", sc[:], I["cvec"], writes=[sc])
            k.act(sc[:], sc[:], AF.Silu, reads=[sc], writes=[sc])
            screp = k.sbuf("m_screp", [128, 16, 128], F32)
            for i in range(16):
                k.ts("dve", screp[:, i, :], onesf, sc[:, i:i + 1], None, ALU.mult, reads=[cmf, sc], writes=[screp])
            vst = k.sbuf("m_vst", [64, 128], F32)
            k.dma("sp", vst[0:48, :], I["ada_b"][l].rearrange("(j p) -> j p", p=128), writes=[vst])
            k.dma("sp", vst[48:56, :], I["norm1_g"][l].rearrange("(j p) -> j p", p=128), writes=[vst])
            k.dma("sp", vst[56:64, :], I["norm2_g"][l].rearrange("(j p) -> j p", p=128), writes=[vst])
            ps = pf()
            k.tr(ps, ps[:, 0:64], vst[:], identf[0:64, 0:64], reads=[vst, cmf])
            vecT = L["vecT"]
            k.cp("dve", vecT[:], ps[:, 0:64], reads=[ps], writes=[vecT])
            modF = L["modF"]
            adab = k.sbuf("m_adab", [1, 2048], F32)
            k.dma("sp", adab[0:1, 0:1024], I["ada_b"][l:l + 1, 2048:3072], writes=[adab])
            k.dma("sp", adab[0:1, 1024:2048], I["ada_b"][l:l + 1, 5120:6144], writes=[adab])
            wring = [k.sbuf(f"m_w{i}", [128, 8, 512], F32) for i in range(2)]
            awv = I["ada_w"][l].rearrange("(kc p) n -> p kc n", p=128)
            for blk in range(12):
                wt = wring[blk % 2]
                for kc in range(8):
                    k.dma("sp" if kc % 2 == 0 else "act", wt[:, kc, :], awv[:, kc, blk * 512:(blk + 1) * 512], writes=[wt])
                ps = pf()
                for jj in range(4):
                    for kc in range(8):
                        k.mm(ps, ps[:, jj * 2:jj * 2 + 2], wt[:, kc, jj * 128:(jj + 1) * 128], sc[:, kc::8],
                             start=(kc == 0), stop=(kc == 7), reads=[wt, sc])
                k.tt("dve", modF[:, blk * 4:(blk + 1) * 4, :], ps[:, 0:8].rearrange("p (a b) -> p a b", b=2),
                     vecT[:, blk * 4:(blk + 1) * 4].unsqueeze(2).to_broadcast([128, 4, 2]), ALU.add,
                     reads=[ps, vecT], writes=[modF])
                if blk in (4, 5, 10, 11):
                    which = 0 if blk < 6 else 1
                    half = blk % 2
                    for s in range(2):
                        p2 = pf()
                        for kc in range(8):
                            k.mm(p2, p2[:, 0:512], screp[:, s * 8 + kc, :], wt[:, kc, :], start=(kc == 0), stop=False, reads=[screp, wt])
                        k.mm(p2, p2[:, 0:512], onesf[0:1, :], adab[0:1, which * 1024 + half * 512: which * 1024 + (half + 1) * 512],
                             start=False, stop=True, reads=[cmf, adab])
                        g = L["grow"][which][s]
                        k.cp("act", g[:, half * 512:(half + 1) * 512], p2[:, 0:512], reads=[p2], writes=[g])
            for wi, (scb, ngb) in enumerate(((8, 48), (32, 56))):
                k.stt(L["mulA"][wi][:], modF[:, scb:scb + 8, :], 1.0, vecT[:, ngb:ngb + 8].unsqueeze(2).to_broadcast([128, 8, 2]),
                      ALU.add, ALU.mult, reads=[modF, vecT], writes=[L["mulA"][wi]])

    def pass_a(L, wi, tiles, src, dst_col0=0, hdst=None):
        hT = H["hT"]
        hdst = hdst or hT
        shb = 0 if wi == 0 else 24
        with k.scope():
            xts = [k.sbuf(f"a_x{i}", [128, 1024], F32) for i in range(2)]
            junk = k.sbuf("a_junk", [128, 1024], BF16)
            xn = [k.sbuf(f"a_xn{i}", [128, 1024], BF16) for i in range(2)]
            ss = [k.sbuf(f"a_ss{i}", [128, 1], F32) for i in range(2)]
            for n, i in enumerate(tiles):
                s = 1 if i < 2 else 0
                xt = xts[n % 2]
                k.dma("sp", xt[:], src[i * 128:(i + 1) * 128, :], reads=[src.sub(i)], writes=[xt])
                s_ = ss[n % 2]
                k.act(junk[:], xt[:], AF.Square, reads=[xt], writes=[junk, s_], accum_out=s_[:])
                k.act(s_[:], s_[:], AF.Sqrt, reads=[s_], writes=[s_], scale=1.0 / 1024, bias=EPS)
                k.op("dve", lambda e, s_=s_: e.reciprocal(out=s_[:], in_=s_[:]), reads=[s_], writes=[s_])
                x_ = xn[n % 2]
                k.ts("dve", x_[:], xt[:], s_[:, 0:1], None, ALU.mult, reads=[xt, s_], writes=[x_])
                p = pb()
                for j in range(8):
                    k.tr(p, p[:, j * 128:(j + 1) * 128], x_[:, j * 128:(j + 1) * 128], identb, reads=[x_, cmb])
                c0 = dst_col0 + n * 128 if hdst is not hT else i * 128
                key = i if hdst is hT else n
                for j in range(8):
                    o = hdst[:, j, c0:c0 + 128]
                    if j % 2 == 0:
                        k.act(o, p[:, j * 128:(j + 1) * 128], AF.Identity, reads=[p, L["mulA"][wi], L["modF"]], writes=[hdst.sub(key)],
                              scale=L["mulA"][wi][:, j, s:s + 1], bias=L["modF"][:, shb + j, s:s + 1])
                    else:
                        k.ts("dve", o, p[:, j * 128:(j + 1) * 128], L["mulA"][wi][:, j, s:s + 1], L["modF"][:, shb + j, s:s + 1],
                             ALU.mult, ALU.add, reads=[p, L["mulA"][wi], L["modF"]], writes=[hdst.sub(key)])

    def hreads(i, n=1):
        return [H["hT"].sub(i + a) for a in range(n)]

    def stage_gla():
        with k.scope():
            W = k.sbuf("g_W", [128, 8, 1568], BF16)
            for kc in range(8):
                k.dma("pool", W[:, kc, :], I["ab_w_in"][0, kc * 128:(kc + 1) * 128, 0:1568], writes=[W])
            wa2f = k.sbuf("g_wa2f", [32, 2, 256], F32)
            k.memset("dve", wa2f[:], 0.0, writes=[wa2f])
            k.dma("sp", wa2f[0:16, 0, :], I["gla_wa"][0, 0], writes=[wa2f])
            k.dma("sp", wa2f[16:32, 1, :], I["gla_wa"][0, 1], writes=[wa2f])
            wa2 = k.sbuf("g_wa2", [32, 2, 256], BF16)
            k.cp("dve", wa2[:], wa2f[:], reads=[wa2f], writes=[wa2])
            baf = k.sbuf("g_baf", [1, 2, 256], F32)
            k.dma("sp", baf[:], I["gla_ba"][0:1], writes=[baf])
            bab = k.sbuf("g_bab", [1, 2, 256], BF16)
            k.cp("dve", bab[:], baf[:], reads=[baf], writes=[bab])
            normg = k.sbuf("g_ng", [128, 128], F32)
            k.dma("sp", normg[:], I["gla_norm_g"][0].partition_broadcast(128), writes=[normg])
            gmf = k.sbuf("g_gm", [128, 258], F32)
            k.dma("sp", gmf[:], I["gm"], writes=[gmf])
            mask4 = [k.sbuf(f"g_mask{d}", [128, 4, 128], BF16) for d in range(2)]
            for d in range(2):
                for h in range(4):
                    k.cp("dve", mask4[d][:, h, :], cmf[:, 1 + d, :], reads=[cmf], writes=[mask4[d]])
            S32 = [k.sbuf(f"g_S32{c}", [128, 256], F32) for c in range(2)]
            Sbf = [k.sbuf(f"g_Sbf{c}", [128, 256], BF16) for c in range(2)]
            lowT = k.sbuf("g_lowT", [32, 128], BF16)
            e1 = k.sbuf("g_e1", [128, 256], F32)
            l1 = k.sbuf("g_l1", [128, 256], BF16)
            EpT = k.sbuf("g_EpT", [128, 256], F32)
            EmT = k.sbuf("g_EmT", [128, 256], F32)
            Ec = k.sbuf("g_Ec", [128, 256], F32)
            qdT = k.sbuf("g_qdT", [128, 256], BF16)
            kiT = k.sbuf("g_kiT", [128, 4, 128], BF16)
            kst = k.sbuf("g_kst", [128, 256], BF16)
            vb = k.sbuf("g_vb", [128, 512], BF16)
            ST = k.sbuf("g_ST", [128, 512], BF16)
            o32 = [k.sbuf(f"g_o32{i}", [128, 512], F32) for i in range(2)]
            of_in = [k.sbuf(f"g_ofin{i}", [128, 512], F32) for i in range(2)]
            sg = k.sbuf("g_sg", [128, 512], F32)
            junk = k.sbuf("g_junk", [128, 128], BF16)
            ss4 = k.sbuf("g_ss4", [128, 4], F32)
            fin = k.sbuf("g_fin", [128, 512], BF16)
            finT = [k.sbuf(f"g_finT{i}", [128, 4, 128], BF16) for i in range(2)]
            cnt = 0
            for d in range(2):
                order = list(range(NT)) if d == 0 else [1, 0] + list(range(NT - 1, 1, -1))

                if os.environ.get("GLA_LIM"):
                    order = order[:int(os.environ["GLA_LIM"])]
                    if d == 1 and os.environ.get("GLA_D0"):
                        continue
                tri = cmb[:, 1 + d, :]
                sx = cmb[:, 3 + d, :]
                last = 127 if d == 0 else 0
                for c in range(2):
                    k.memset("dve", S32[c][:], 0.0, writes=[S32[c]])
                    k.memset("dve", Sbf[c][:], 0.0, writes=[Sbf[c]])
                for i in order:
                    if GSTEP < 1:
                        continue
                    hr = hreads(i)
                    pq = pf()
                    for g4 in range(4):
                        for kc in range(8):
                            k.mm(pq, pq[:, g4 * 128:(g4 + 1) * 128], W[:, kc, g4 * 128:(g4 + 1) * 128], hTi(kc, i),
                                 start=(kc == 0), stop=(kc == 7), reads=[W] + hr)
                    pl = pf()
                    for kc in range(8):
                        k.mm(pl, pl[0:32, 0:128], W[:, kc, 1536:1568], hTi(kc, i), start=(kc == 0), stop=(kc == 7), reads=[W] + hr)
                    k.cp("act", lowT[:], pl[0:32, 0:128], reads=[pl], writes=[lowT])
                    if GSTEP < 2:
                        continue
                    pz = pf()
                    k.mm(pz, pz[:, 0:256], lowT[:], wa2[:, d, :], start=True, stop=False, reads=[lowT, wa2])
                    k.mm(pz, pz[:, 0:256], onesb[0:1, :], bab[0:1, d, :], start=False, stop=True, reads=[cmb, bab])
                    k.act(e1[:], pz[:, 0:256], AF.Exp, reads=[pz], writes=[e1], scale=-1.0)
                    k.act(l1[:], e1[:], AF.Ln, reads=[e1], writes=[l1], bias=1.0)
                    if GSTEP < 3:
                        continue
                    pbt = pf()
                    for c in range(2):
                        k.mm(pbt, pbt[:, c * 128:(c + 1) * 128], l1[:, c * 128:(c + 1) * 128], tri, reads=[l1, cmb])
                    k.act(EpT[:], pbt[:, 0:256], AF.Exp, reads=[pbt], writes=[EpT], scale=-1.0 / 16)
                    k.act(EmT[:], pbt[:, 0:256], AF.Exp, reads=[pbt], writes=[EmT], scale=1.0 / 16)
                    k.stt(qdT[:], pq[:, 0:256], 0.125, EpT[:], ALU.mult, ALU.mult, reads=[pq, EpT], writes=[qdT])
                    for h in range(4):
                        c, hh = h // 2, h % 2
                        k.stt(kiT[:, h, :], pq[:, 256 + c * 128:256 + (c + 1) * 128], gmf[:, hh:hh + 1], EmT[:, c * 128:(c + 1) * 128],
                              ALU.mult, ALU.mult, reads=[pq, gmf, EmT], writes=[kiT])
                    if GSTEP < 4:
                        continue
                    pk = pf()
                    pv = pf()
                    for kc in range(8):
                        k.mm(pk, pk[:, 0:256], hTi(kc, i), W[:, kc, 256:512], start=(kc == 0), stop=(kc == 7), reads=[W] + hr)
                    for kc in range(8):
                        k.mm(pv, pv[:, 0:512], hTi(kc, i), W[:, kc, 512:1024], start=(kc == 0), stop=(kc == 7), reads=[W] + hr)
                    pc = pf()
                    k.mm(pc, pc[:, 0:256], sx, l1[:], reads=[cmb, l1])
                    k.act(Ec[:], pc[:, 0:256], AF.Exp, reads=[pc], writes=[Ec], scale=-1.0 / 16)
                    k.tt("dve", kst[:], pk[:, 0:256], Ec[:], ALU.mult, reads=[pk, Ec], writes=[kst])
                    k.cp("act", vb[:], pv[:, 0:512], reads=[pv], writes=[vb])
                    if GSTEP < 5:
                        continue
                    pss = pf()
                    for h in range(4):
                        c = h // 2
                        k.mm(pss, pss[:, h * 128:(h + 1) * 128], kiT[:, h, :], qdT[:, c * 128:(c + 1) * 128], reads=[kiT, qdT])
                    k.tt("dve", ST[:], pss[:, 0:512], mask4[d][:].rearrange("p a b -> p (a b)"), ALU.mult, reads=[pss, mask4[d]], writes=[ST])
                    if GSTEP < 6:
                        continue
                    po = pf()
                    for h in range(4):
                        c, hh = h // 2, h % 2
                        k.mm(po, po[:, h * 128:(h + 1) * 128], qdT[:, c * 128:(c + 1) * 128], Sbf[c][:, hh * 128:(hh + 1) * 128],
                             start=True, stop=False, reads=[qdT, Sbf[c]])
                        k.mm(po, po[:, h * 128:(h + 1) * 128], ST[:, h * 128:(h + 1) * 128], vb[:, h * 128:(h + 1) * 128],
                             start=False, stop=True, reads=[ST, vb])
                    if GSTEP < 7:
                        continue
                    pst = pf()
                    for c in range(2):
                        k.mm(pst, pst[:, c * 256:(c + 1) * 256], kst[:, c * 128:(c + 1) * 128], vb[:, c * 256:(c + 1) * 256], reads=[kst, vb])
                    for c in range(2):
                        k.stt(S32[c][:], S32[c][:], EpT[:, c * 128 + last:c * 128 + last + 1], pst[:, c * 256:(c + 1) * 256], ALU.mult, ALU.add,
                              reads=[S32[c], EpT, pst], writes=[S32[c]])
                        k.tt("pool", S32[c][:], S32[c][:], gmf[:, 2:258], ALU.mult, reads=[S32[c], gmf], writes=[S32[c]])
                        k.cp("pool", Sbf[c][:], S32[c][:], reads=[S32[c]], writes=[Sbf[c]])
                    if GSTEP < 8:
                        continue
                    cnt += 1
                    if d == 0:
                        o_ = o32[cnt % 2]
                        k.cp("act", o_[:], po[:, 0:512], reads=[po], writes=[o_])
                        k.dma("sp", ofD[i * 128:(i + 1) * 128, 0:512], o_[:], reads=[o_], writes=[ofD.sub(i)])
                    else:
                        oi = of_in[cnt % 2]
                        k.dma("sp", oi[:], ofD[i * 128:(i + 1) * 128, 0:512], reads=[ofD.sub(i)], writes=[oi])
                        o_ = o32[cnt % 2]
                        k.tt("dve", o_[:], po[:, 0:512], oi[:], ALU.add, reads=[po, oi], writes=[o_])
                        for h in range(4):
                            k.act(junk[:], o_[:, h * 128:(h + 1) * 128], AF.Square, reads=[o_], writes=[junk, ss4], accum_out=ss4[:, h:h + 1])
                        k.act(ss4[:], ss4[:], AF.Sqrt, reads=[ss4], writes=[ss4], scale=1.0 / 128, bias=EPS)
                        k.op("dve", lambda e: e.reciprocal(out=ss4[:], in_=ss4[:]), reads=[ss4], writes=[ss4])
                        pg = pf()
                        for kc in range(8):
                            k.mm(pg, pg[:, 0:512], hTi(kc, i), W[:, kc, 1024:1536], start=(kc == 0), stop=(kc == 7), reads=[W] + hr)
                        k.act(sg[:], pg[:, 0:512], AF.Silu, reads=[pg], writes=[sg])
                        for h in range(4):
                            k.stt(o_[:, h * 128:(h + 1) * 128], o_[:, h * 128:(h + 1) * 128], ss4[:, h:h + 1], normg[:], ALU.mult, ALU.mult,
                                  reads=[o_, ss4, normg], writes=[o_])
                        k.tt("dve", fin[:], o_[:], sg[:], ALU.mult, reads=[o_, sg], writes=[fin])
                        p = pb()
                        for c in range(4):
                            k.tr(p, p[:, c * 128:(c + 1) * 128], fin[:, c * 128:(c + 1) * 128], identb, reads=[fin, cmb])
                        ft = finT[cnt % 2]
                        k.cp("act", ft[:].rearrange("p a b -> p (a b)"), p[:, 0:512], reads=[p], writes=[ft])
                        k.dma("sp", mixT[0:512, i * 128:(i + 1) * 128].rearrange("(c p) t -> p c t", p=128), ft[:],
                              reads=[ft], writes=[mixT.sub(("g", i))])

    def stage_s5():
        with k.scope():
            pst_ = k.sbuf("s_pst", [96, 128], F32)
            k.dma("sp", pst_[0:32, :], I["s5_lam_re"][0].rearrange("d (st gl) p -> (d st) (gl p)", gl=2), writes=[pst_])
            k.dma("sp", pst_[32:64, :], I["s5_lam_im"][0].rearrange("d (st gl) p -> (d st) (gl p)", gl=2), writes=[pst_])
            lsm = k.sbuf("s_lsm", [96, 2], F32)
            k.dma("sp", lsm[64:96, :], I["s5_log_step"][0].rearrange("d (st gl) -> (d st) gl", gl=2), writes=[lsm])
            for gl in range(2):
                k.cp("dve", pst_[64:96, gl * 64:(gl + 1) * 64], lsm[64:96, gl:gl + 1].to_broadcast([32, 64]), reads=[lsm], writes=[pst_])
            ps = pf()
            k.tr(ps, ps[:, 0:96], pst_[:], identf[0:96, 0:96], reads=[pst_, cmf])
            par = k.sbuf("s_par", [128, 96], F32)
            k.cp("dve", par[:], ps[:, 0:96], reads=[ps], writes=[par])
            lre, lim = par[:, 0:32], par[:, 32:64]
            names = ["step", "mag", "th", "y", "fr", "t1", "t2", "cb", "sb", "lbr", "lbi", "den", "fre", "fim", "rr", "nfim"]
            V = {n: k.sbuf("s_" + n, [128, 32], F32) for n in names}
            yi = k.sbuf("s_yi", [128, 32], I32)

            def dv(out, a, b, op):
                k.tt("dve", V[out][:], a, b, op, reads=[par] + list(V.values()), writes=[V[out]])

            k.act(V["step"][:], par[:, 64:96], AF.Exp, reads=[par], writes=[V["step"]])
            dv("t1", lre, V["step"][:], ALU.mult)
            k.act(V["mag"][:], V["t1"][:], AF.Exp, reads=[V["t1"]], writes=[V["mag"]])
            dv("th", lim, V["step"][:], ALU.mult)
            k.ts("dve", V["y"][:], V["th"][:], 1.0 / (2 * math.pi), None, ALU.mult, reads=[V["th"]], writes=[V["y"]])
            k.cp("dve", yi[:], V["y"][:], reads=[V["y"]], writes=[yi])
            k.cp("dve", V["t1"][:], yi[:], reads=[yi], writes=[V["t1"]])
            dv("fr", V["y"][:], V["t1"][:], ALU.subtract)

            def wrap(name):
                k.ts("dve", V["t1"][:], V[name][:], 0.5, None, ALU.is_gt, reads=[V[name]], writes=[V["t1"]])
                k.ts("dve", V["t2"][:], V[name][:], -0.5, None, ALU.is_lt, reads=[V[name]], writes=[V["t2"]])
                dv(name, V[name][:], V["t1"][:], ALU.subtract)
                dv(name, V[name][:], V["t2"][:], ALU.add)
            wrap("fr")
            k.act(V["sb"][:], V["fr"][:], AF.Sin, reads=[V["fr"]], writes=[V["sb"]], scale=2 * math.pi)
            k.ts("dve", V["fr"][:], V["fr"][:], 0.25, None, ALU.add, reads=[V["fr"]], writes=[V["fr"]])
            wrap("fr")
            k.act(V["cb"][:], V["fr"][:], AF.Sin, reads=[V["fr"]], writes=[V["cb"]], scale=2 * math.pi)
            dv("lbr", V["mag"][:], V["cb"][:], ALU.mult)
            dv("lbi", V["mag"][:], V["sb"][:], ALU.mult)
            dv("t1", lre, lre, ALU.mult)
            dv("t2", lim, lim, ALU.mult)
            dv("den", V["t1"][:], V["t2"][:], ALU.add)
            k.op("dve", lambda e: e.reciprocal(out=V["den"][:], in_=V["den"][:]), reads=[V["den"]], writes=[V["den"]])
            k.ts("dve", V["y"][:], V["lbr"][:], -1.0, None, ALU.add, reads=[V["lbr"]], writes=[V["y"]])
            dv("t1", V["y"][:], lre, ALU.mult)
            dv("t2", V["lbi"][:], lim, ALU.mult)
            dv("fre", V["t1"][:], V["t2"][:], ALU.add)
            dv("fre", V["fre"][:], V["den"][:], ALU.mult)
            dv("t1", V["lbi"][:], lre, ALU.mult)
            dv("t2", V["y"][:], lim, ALU.mult)
            dv("fim", V["t1"][:], V["t2"][:], ALU.subtract)
            dv("fim", V["fim"][:], V["den"][:], ALU.mult)
            k.ts("dve", V["nfim"][:], V["fim"][:], -1.0, None, ALU.mult, reads=[V["fim"]], writes=[V["nfim"]])
            if os.environ.get("S5_DUMP") == "1":
                dd = k.sbuf("s_dd", [128, 4, 256], F32)
                k.memset("dve", dd[:], 0.0, writes=[dd])
                for ii, nm in enumerate(["fre", "fim", "mag", "cb", "sb", "th", "step"]):
                    k.cp("dve", dd[:, 0, ii * 32:(ii + 1) * 32], V[nm][:], reads=[V[nm]], writes=[dd])
                k.dma("sp", dbg_out, dd[:], reads=[dd], writes=[dbgT])
                return
            rt = k.sbuf("s_rt", [128, 16, 128], F32)
            ctab = k.sbuf("s_ctab", [128, 16, 128], F32)
            stab = k.sbuf("s_stab", [128, 16, 2, 128], F32)
            cL_t = k.sbuf("s_cL", [128, 32], F32)
            sL_t = k.sbuf("s_sL", [128, 32], F32)
            nsL_t = k.sbuf("s_nsL", [128, 32], F32)
            thn = k.sbuf("s_thn", [128, 32], F32)
            k.ts("dve", thn[:], V["th"][:], 1.0 / (2 * math.pi), None, ALU.mult, reads=[V["th"]], writes=[thn])
            iot = k.sbuf("s_iot", [128, 128], F32)
            k.ts("dve", iot[:], cmf[:, 6, :], -1.0, None, ALU.add, reads=[cmf], writes=[iot])
            A = {n_: k.sbuf("s_A" + n_, [128, 128], F32) for n_ in ("y", "t1", "t2")}
            Ai = k.sbuf("s_Ai", [128, 128], I32)

            def sincos(y_in, w, cos_out, sin_out, rd):
                y, t1, t2 = A["y"][:, 0:w], A["t1"][:, 0:w], A["t2"][:, 0:w]
                allA = list(A.values())
                k.cp("dve", Ai[:, 0:w], y_in, reads=rd, writes=[Ai])
                k.cp("dve", t1, Ai[:, 0:w], reads=[Ai], writes=[A["t1"]])
                k.tt("dve", y, y_in, t1, ALU.subtract, reads=rd + allA, writes=[A["y"]])

                def wrap_():
                    k.ts("dve", t1, y, 0.5, None, ALU.is_gt, reads=allA, writes=[A["t1"]])
                    k.ts("dve", t2, y, -0.5, None, ALU.is_lt, reads=allA, writes=[A["t2"]])
                    k.tt("dve", y, y, t1, ALU.subtract, reads=allA, writes=[A["y"]])
                    k.tt("dve", y, y, t2, ALU.add, reads=allA, writes=[A["y"]])
                wrap_()
                k.act(sin_out, y, AF.Sin, reads=allA, writes=rd, scale=2 * math.pi)
                k.ts("dve", y, y, 0.25, None, ALU.add, reads=allA, writes=[A["y"]])
                wrap_()
                k.act(cos_out, y, AF.Sin, reads=allA, writes=rd, scale=2 * math.pi)

            th128 = k.sbuf("s_th128", [128, 32], F32)
            k.ts("dve", th128[:], thn[:], 128.0, None, ALU.mult, reads=[thn], writes=[th128])
            sincos(th128[:], 32, cL_t[:], sL_t[:], [th128, cL_t, sL_t])
            k.ts("dve", nsL_t[:], sL_t[:], -1.0, None, ALU.mult, reads=[sL_t], writes=[nsL_t])
            cL, sL, nsL = cL_t[:], sL_t[:], nsL_t[:]
            ck, sk, nsk = cL_t, sL_t, nsL_t
            ang = k.sbuf("s_ang", [128, 128], F32)
            Bre = k.sbuf("s_Bre", [128, 16, 16], F32)
            Bim = k.sbuf("s_Bim", [128, 16, 16], F32)
            k.dma("sp", Bre[:], I["s5_b_re"][0].rearrange("(st gl) p c -> (gl p) st c", gl=2), writes=[Bre])
            k.dma("sp", Bim[:], I["s5_b_im"][0].rearrange("(st gl) p c -> (gl p) st c", gl=2), writes=[Bim])
            s5mf = k.sbuf("s_msk", [128, 4, 128], F32)
            k.dma("sp", s5mf[:], I["s5m"], writes=[s5mf])
            drv = k.sbuf("s_drv", [128, 16, 2, 128], BF16)
            rdo = k.sbuf("s_rdo", [128, 16, 2, 128], BF16)
            bx = [k.sbuf(f"s_bx{i}", [128, 8, 16], F32) for i in range(2)]
            bx2 = [k.sbuf(f"s_bx2{i}", [128, 8, 16], F32) for i in range(2)]
            Y = [k.sbuf(f"s_Y{i}", [128, 4, 128], F32) for i in range(2)]
            gcnt = [0]

            def gen_dir(d):
                for st in range(16):
                    ds = d * 16 + st
                    k.ts("dve", rt[:, st, :], onesf, V["mag"][:, ds:ds + 1], None, ALU.mult, reads=[cmf, V["mag"]], writes=[rt])
                for st in range(16):
                    ds = d * 16 + st
                    k.ts("dve", ang[:], iot[:], thn[:, ds:ds + 1], None, ALU.mult, reads=[iot, thn], writes=[ang])
                    sincos(ang[:], 128, ctab[:, st, :], stab[:, st, 0, :], [ang, ctab, stab])
                k.ts("dve", stab[:, :, 1, :], stab[:, :, 0, :], -1.0, None, ALU.mult, reads=[stab], writes=[stab])
                for st in range(16):
                    ds = d * 16 + st
                    q = st % 4
                    for ri in range(2):
                        gcnt[0] += 1
                        b_, b2_ = bx[gcnt[0] % 2], bx2[gcnt[0] % 2]
                        B1 = (Bre if ri == 0 else Bim)[:, st, :].unsqueeze(1).to_broadcast([128, 8, 16])
                        B2 = (Bim if ri == 0 else Bre)[:, st, :].unsqueeze(1).to_broadcast([128, 8, 16])
                        f2 = V["nfim"] if ri == 0 else V["fim"]
                        k.ts("dve", b_[:], B1, V["fre"][:, ds:ds + 1], None, ALU.mult, reads=[Bre, Bim, V["fre"]], writes=[b_])
                        k.stt(b2_[:], B2, f2[:, ds:ds + 1], b_[:], ALU.mult, ALU.add, reads=[Bre, Bim, f2, b_], writes=[b2_])
                        k.tt("dve", b_[:].rearrange("p a b -> p (a b)"), b2_[:].rearrange("p a b -> p (a b)"), s5mf[:, q, :], ALU.mult,
                             reads=[b2_, s5mf], writes=[b_])
                        p_ = pf()
                        k.tr(p_, p_[:, 0:128], b_[:].rearrange("p a b -> p (a b)"), identf, reads=[b_, cmf])
                        k.cp("act", drv[:, st, ri, :], p_[:, 0:128], reads=[p_], writes=[drv])
                for oc in range(4):
                    for ri in range(2):
                        gcnt[0] += 1
                        y_ = Y[gcnt[0] % 2]
                        k.memset("dve", y_[:], 0.0, writes=[y_])
                        src = I["s5_c_re" if ri == 0 else "s5_c_im"]
                        for gi in range(8):
                            g = oc * 8 + gi
                            k.dma("sp", y_[gi * 16:(gi + 1) * 16, gi // 2, (gi % 2) * 64:(gi % 2 + 1) * 64], src[0, d, g], writes=[y_])
                        for s4 in range(4):
                            st = oc * 4 + s4
                            p_ = pf()
                            k.tr(p_, p_[:, 0:128], y_[:, s4, :], identf, reads=[y_, cmf])
                            if ri == 0:
                                k.cp("act", rdo[:, st, 0, :], p_[:, 0:128], reads=[p_], writes=[rdo])
                            else:
                                k.ts("dve", rdo[:, st, 1, :], p_[:, 0:128], -1.0, None, ALU.mult, reads=[p_], writes=[rdo])
            Wu = k.sbuf("s_Wu", [128, 8, 512], BF16)
            for kc in range(8):
                k.dma("pool", Wu[:, kc, :], I["ab_w_in"][0, kc * 128:(kc + 1) * 128, 1568:2080], writes=[Wu])
            glw = k.sbuf("s_glw", [128, 4, 512], BF16)
            k.dma("pool", glw[:], I["s5_glu_w"][0].rearrange("(kc p) n -> p kc n", p=128), writes=[glw])
            v3 = k.sbuf("s_v3", [8, 128], F32)
            k.dma("sp", v3[0:4, :], I["s5_d"][0].rearrange("(j p) -> j p", p=128), writes=[v3])
            k.dma("sp", v3[4:8, :], I["s5_glu_b"][0].rearrange("(j p) -> j p", p=128), writes=[v3])
            p_ = pf()
            k.tr(p_, p_[:, 0:8], v3[:], identf[0:8, 0:8], reads=[v3, cmf])
            dgb = k.sbuf("s_dgb", [128, 8], F32)
            k.cp("dve", dgb[:], p_[:, 0:8], reads=[p_], writes=[dgb])
            uT = [k.sbuf(f"s_uT{i}", [128, 4, 128], BF16) for i in range(2)]
            bt = [k.sbuf(f"s_bt{i}", [128, 2, 128], F32) for i in range(2)]
            t1_ = [k.sbuf(f"s_t1{i}", [128, 2, 128], F32) for i in range(2)]
            t2_ = [k.sbuf(f"s_t2{i}", [128, 2, 128], F32) for i in range(2)]
            wv = [k.sbuf(f"s_w{i}", [128, 2, 128], F32) for i in range(2)]
            o1 = [k.sbuf(f"s_o1{i}", [128, 2, 128], F32) for i in range(2)]
            o2 = [k.sbuf(f"s_o2{i}", [128, 2, 128], F32) for i in range(2)]
            sri = [k.sbuf(f"s_sri{i}", [128, 4, 2, 128], BF16) for i in range(2)]
            winit = k.sbuf("s_winit", [128, 32, 2], F32)
            wtmp = k.sbuf("s_wtmp", [128, 2], F32)
            yf32 = [k.sbuf(f"s_yf{i}", [128, 4, 128], F32) for i in range(2)]
            yin = [k.sbuf(f"s_yin{i}", [128, 4, 128], F32) for i in range(2)]
            yy = k.sbuf("s_yy", [128, 4, 128], F32)
            q1 = k.sbuf("s_q1", [128, 4, 128], F32)
            yg = k.sbuf("s_yg", [128, 4, 128], BF16)
            yo = [k.sbuf(f"s_yo{i}", [128, 4, 128], BF16) for i in range(2)]
            k.memset("dve", winit[:], 0.0, writes=[winit])
            n = 0
            for d in range(2):
                order = list(range(NT)) if d == 0 else [1, 0] + list(range(NT - 1, 1, -1))

                if os.environ.get("GLA_LIM"):
                    order = order[:int(os.environ["GLA_LIM"])]
                sl = slice(None, None, None) if d == 0 else slice(None, None, -1)
                last = 127 if d == 0 else 0
                gen_dir(d)
                for i in order:
                    hr = hreads(i)
                    n += 1
                    u_ = uT[n % 2]
                    pu = pf()
                    for c in range(4):
                        for kc in range(8):
                            k.mm(pu, pu[:, c * 128:(c + 1) * 128], Wu[:, kc, c * 128:(c + 1) * 128], hTi(kc, i),
                                 start=(kc == 0), stop=(kc == 7), reads=[Wu] + hr)
                    k.cp("act", u_[:].rearrange("p a b -> p (a b)"), pu[:, 0:512], reads=[pu], writes=[u_])
                    py = PX
                    for oc in range(4):
                        s_ = sri[(n * 4 + oc) % 2]
                        for s4 in range(4):
                            st = oc * 4 + s4
                            ds = d * 16 + st
                            m = (n * 16 + st) % 2
                            pd = pf()
                            for ri in range(2):
                                k.mm(pd, pd[:, ri * 128:(ri + 1) * 128], drv[:, st, ri, :], u_[:, oc, :], reads=[drv, u_])
                            if os.environ.get("S5_DUMP") == "4" and d == 0 and i == 0 and st == 0:
                                k.cp("dve", yy[:, 0:2, :].rearrange("p a b -> p (a b)"), pd[:, 0:256], reads=[pd], writes=[yy])
                                k.dma("sp", dbg_out[:, 0, :], yy[:, 0:2, :].rearrange("p a b -> p (a b)"), reads=[yy], writes=[dbgT])
                            P3 = pd[:, 0:256].rearrange("p (a b) -> p a b", a=2)
                            ctb = ctab[:, st, sl].unsqueeze(1).to_broadcast([128, 2, 128])
                            stb = stab[:, st, :, sl]
                            k.tt("dve", t1_[m][:], P3, ctb, ALU.mult, reads=[pd, ctab], writes=[t1_[m]])
                            k.tt("dve", t2_[m][:], P3[:, ::-1, :], stb, ALU.mult, reads=[pd, stab], writes=[t2_[m]])
                            k.tt("pool", bt[m][:], t1_[m][:], t2_[m][:], ALU.add, reads=[t1_[m], t2_[m]], writes=[bt[m]])
                            for ri in range(2):
                                k.op("dve", lambda e, m=m, ri=ri, ds=ds, st=st, sl=sl: e.tensor_tensor_scan(
                                    out=wv[m][:, ri, sl], data0=rt[:, st, :], data1=bt[m][:, ri, sl], initial=winit[:, ds, ri:ri + 1],
                                    op0=ALU.mult, op1=ALU.add), reads=[rt, bt[m], winit], writes=[wv[m]])
                            if os.environ.get("S5_DUMP") == "4" and d == 0 and i == 0 and st == 0:
                                k.dma("sp", dbg_out[:, 2, :], bt[m][:].rearrange("p a b -> p (a b)"), reads=[bt[m]], writes=[dbgT])
                                k.dma("sp", dbg_out[:, 3, :], wv[m][:].rearrange("p a b -> p (a b)"), reads=[wv[m]], writes=[dbgT])
                            k.ts("dve", wtmp[:, 0:1], wv[m][:, 0, last:last + 1], cL[:, ds:ds + 1], None, ALU.mult, reads=[wv[m], ck], writes=[wtmp])
                            k.ts("dve", wtmp[:, 1:2], wv[m][:, 1, last:last + 1], cL[:, ds:ds + 1], None, ALU.mult, reads=[wv[m], ck], writes=[wtmp])
                            k.stt(winit[:, ds, 0:1], wv[m][:, 1, last:last + 1], nsL[:, ds:ds + 1], wtmp[:, 0:1], ALU.mult, ALU.add,
                                  reads=[wv[m], nsk, wtmp], writes=[winit])
                            k.stt(winit[:, ds, 1:2], wv[m][:, 0, last:last + 1], sL[:, ds:ds + 1], wtmp[:, 1:2], ALU.mult, ALU.add,
                                  reads=[wv[m], sk, wtmp], writes=[winit])
                            k.tt("pool", o1[m][:], wv[m][:], ctb, ALU.mult, reads=[wv[m], ctab], writes=[o1[m]])
                            k.tt("pool", o2[m][:], wv[m][:, ::-1, :], stb, ALU.mult, reads=[wv[m], stab], writes=[o2[m]])
                            k.tt("pool", s_[:, s4, :, :], o1[m][:], o2[m][:], ALU.subtract, reads=[o1[m], o2[m]], writes=[s_])
                            if os.environ.get("S5_DUMP") == "4" and d == 0 and i == 0 and st == 0:
                                k.tt("pool", yy[:, 2:4, :], o1[m][:], o2[m][:], ALU.subtract, reads=[o1[m], o2[m]], writes=[yy])
                                k.dma("sp", dbg_out[:, 1, :], yy[:, 2:4, :].rearrange("p a b -> p (a b)"), reads=[yy], writes=[dbgT])
                        for s4 in range(4):
                            for ri in range(2):
                                k.mm(py, py[:, oc * 128:(oc + 1) * 128], rdo[:, oc * 4 + s4, ri, :], s_[:, s4, ri, :],
                                     start=(s4 == 0 and ri == 0), stop=(s4 == 3 and ri == 1), reads=[rdo, s_])
                    if d == 0:
                        y_ = yf32[n % 2]
                        k.cp("act", y_[:].rearrange("p a b -> p (a b)"), py[:, 0:512], reads=[py], writes=[y_])
                        k.dma("sp", yfD[:, i * 128:(i + 1) * 128].rearrange("(c p) t -> p c t", p=128), y_[:], reads=[y_], writes=[yfD.sub(i)])
                        if os.environ.get("S5_DUMP") == "3" and i < 2:
                            k.dma("sp", dbg_out[:, :, i * 128:(i + 1) * 128], y_[:], reads=[y_], writes=[dbgT])
                    else:
                        yi_ = yin[n % 2]
                        k.dma("sp", yi_[:], yfD[:, i * 128:(i + 1) * 128].rearrange("(c p) t -> p c t", p=128), reads=[yfD.sub(i)], writes=[yi_])
                        k.tt("dve", yy[:].rearrange("p a b -> p (a b)"), py[:, 0:512], yi_[:].rearrange("p a b -> p (a b)"), ALU.add,
                             reads=[py, yi_], writes=[yy])
                        for c in range(4):
                            k.stt(yy[:, c, :], u_[:, c, :], dgb[:, c:c + 1], yy[:, c, :], ALU.mult, ALU.add, reads=[u_, dgb, yy], writes=[yy])
                        if os.environ.get("S5_DUMP") == "2" and i < 2:
                            k.dma("sp", dbg_out[:, :, i * 128:(i + 1) * 128], yy[:], reads=[yy], writes=[dbgT])
                        yf = yy[:].rearrange("p a b -> p (a b)")
                        qf = q1[:].rearrange("p a b -> p (a b)")
                        k.tt("pool", qf, yf, yf, ALU.mult, reads=[yy], writes=[q1])
                        k.ts("dve", qf, qf, 0.044715, 1.0, ALU.mult, ALU.add, reads=[q1], writes=[q1])
                        k.tt("dve", qf, qf, yf, ALU.mult, reads=[q1, yy], writes=[q1])
                        k.act(qf, qf, AF.Sigmoid, reads=[q1], writes=[q1], scale=1.5957691216)
                        k.tt("dve", yf, yf, qf, ALU.mult, reads=[yy, q1], writes=[yy])
                        k.cp("pool", yg[:], yy[:], reads=[yy], writes=[yg])
                        pz = pf()
                        for c in range(4):
                            for kc in range(4):
                                k.mm(pz, pz[:, c * 128:(c + 1) * 128], glw[:, kc, c * 128:(c + 1) * 128], yg[:, kc, :],
                                     start=(kc == 0), stop=(kc == 3), reads=[glw, yg])
                        for c in range(4):
                            k.act(q1[:, c, :], pz[:, c * 128:(c + 1) * 128], AF.Sigmoid, reads=[pz, dgb], writes=[q1], bias=dgb[:, 4 + c:5 + c])
                        yo_ = yo[n % 2]
                        k.tt("dve", yo_[:].rearrange("p a b -> p (a b)"), yf, qf, ALU.mult, reads=[yy, q1], writes=[yo_])
                        k.dma("sp", mixT[512:1024, i * 128:(i + 1) * 128].rearrange("(c p) t -> p c t", p=128), yo_[:],
                              reads=[yo_], writes=[mixT.sub(("s", i))])
                if d == 0:
                    k.memset("dve", winit[:], 0.0, writes=[winit])

    def stage_outproj(l, L, wname, nk, tiles, src, mixkeys):
        with k.scope():
            Wo = k.sbuf("o_W", [128, nk, 1024], BF16)
            for kc in range(nk):
                k.dma("pool", Wo[:, kc, :], I[wname][0, kc * 128:(kc + 1) * 128, :], writes=[Wo])
            mt = [k.sbuf(f"o_m{i}", [128, nk, 128], BF16) for i in range(2)]
            xt = [k.sbuf(f"o_x{i}", [128, 1024], F32) for i in range(2)]
            for n, i in enumerate(tiles):
                s = 1 if i < 2 else 0
                m_, x_ = mt[n % 2], xt[n % 2]
                k.dma("sp", m_[:], mixT[0:nk * 128, i * 128:(i + 1) * 128].rearrange("(c p) t -> p c t", p=128),
                      reads=[mixT.sub((key, i)) for key in mixkeys], writes=[m_])
                k.dma("act", x_[:], src[i * 128:(i + 1) * 128, :], reads=[src.sub(i)], writes=[x_])
                for hf in range(2):
                    p_ = pf()
                    for kc in range(nk):
                        k.mm(p_, p_[:, 0:512], m_[:, kc, :], Wo[:, kc, hf * 512:(hf + 1) * 512], start=(kc == 0), stop=(kc == nk - 1), reads=[m_, Wo])
                    g = L["grow"][0][s]
                    k.tt("dve", p_[:, 0:512], p_[:, 0:512], g[:, hf * 512:(hf + 1) * 512], ALU.mult, reads=[p_, g], writes=[p_])
                    k.tt("dve", x_[:, hf * 512:(hf + 1) * 512], x_[:, hf * 512:(hf + 1) * 512], p_[:, 0:512], ALU.add, reads=[x_, p_], writes=[x_])
                k.dma("sp", xres[i * 128:(i + 1) * 128, :], x_[:], reads=[x_], writes=[xres.sub(i)])

    def stage_moe(l, L, tiles, final):
        with k.scope():
            lim = int(os.environ.get("MOE_E", "32"))
            BT = 12
            G = k.sbuf("e_G", [128, BT, 32], F32)
            wr = k.sbuf("e_wr", [128, 8, 32], BF16)
            k.dma("pool", wr[:], I["moe_w_router"][l].rearrange("(kc p) n -> p kc n", p=128), writes=[wr])
            brow = k.sbuf("e_brow", [128, 32], F32)
            k.dma("sp", brow[:], I["moe_b_router"][l].partition_broadcast(128), writes=[brow])
            lg = k.sbuf("e_lg", [128, 32], F32)
            m8 = k.sbuf("e_m8", [128, 8], F32)
            nm = k.sbuf("e_nm", [128, 1], F32)
            msk = k.sbuf("e_msk", [128, 32], F32)
            ex = k.sbuf("e_ex", [128, 32], F32)
            den = k.sbuf("e_den", [128, 1], F32)
            bguT = k.sbuf("e_bguT", [128, 512], F32)
            with k.scope():
                bst = k.sbuf("e_bst", [128, 4, 128], F32)
                k.dma("sp", bst[:], I["moe_b_gu"][l].rearrange("e (j p) -> (e j) p", p=128).rearrange("(a r) p -> r a p", r=128), writes=[bst])
                for a_i in range(4):
                    p_ = pf()
                    k.tr(p_, p_[:, 0:128], bst[:, a_i, :], identf, reads=[bst, cmf])
                    k.cp("dve", bguT[:, a_i * 128:(a_i + 1) * 128], p_[:, 0:128], reads=[p_], writes=[bguT])
            bd = k.sbuf("e_bd", [32, 1024], F32)
            k.dma("sp", bd[:], I["moe_b_down"][l], writes=[bd])
            NS = 8
            slabs = [k.sbuf(f"e_sl{i}", [128, 8, 512], BF16) for i in range(NS)]
            acc = k.sbuf("e_acc", [128, BT, 1024], F32)
            hTb = k.sbuf("e_hTb", [128, 8, BT * 128], BF16)
            actT = k.sbuf("e_actT", [128, 8, 512], BF16)
            gc = k.sbuf("e_gc", [128, 512], F32)
            sgm = k.sbuf("e_sg", [128, 512], F32)
            lc = k.sbuf("e_lc", [128, 512], F32)
            GT = k.sbuf("e_GT", [32, 128], F32)
            xt = k.sbuf("e_x", [128, 1024], F32)
            junk = k.sbuf("e_junk", [128, 1024], BF16)
            ssq = k.sbuf("e_ssq", [128, 1], F32)
            fng = None
            if final:
                fng = k.sbuf("e_fng", [128, 1024], F32)
                k.dma("sp", fng[:], I["final_norm_g"].partition_broadcast(128), writes=[fng])
            blocks = [tiles[a_:a_ + BT] for a_ in range(0, len(tiles), BT)]
            scnt = 0
            for blk in blocks:
                pass_a(L, 1, blk, xres, hdst=hTb)
                for bi, i in enumerate(blk):
                    p_ = pf()
                    for kc in range(8):
                        k.mm(p_, p_[:, 0:32], hTb[:, kc, bi * 128:(bi + 1) * 128], wr[:, kc, :], start=(kc == 0), stop=(kc == 7), reads=[wr, hTb.sub(bi)])
                    k.tt("dve", lg[:], p_[:, 0:32], brow[:], ALU.add, reads=[p_, brow], writes=[lg])
                    k.op("dve", lambda e: e.max(out=m8[:], in_=lg[:]), reads=[lg], writes=[m8])
                    k.ts("dve", msk[:], lg[:], m8[:, 3:4], None, ALU.is_ge, reads=[lg, m8], writes=[msk])
                    k.ts("dve", nm[:], m8[:, 0:1], -1.0, None, ALU.mult, reads=[m8], writes=[nm])
                    k.act(ex[:], lg[:], AF.Exp, reads=[lg, nm], writes=[ex], bias=nm[:, 0:1])
                    k.tt("dve", ex[:], ex[:], msk[:], ALU.mult, reads=[ex, msk], writes=[ex])
                    k.op("dve", lambda e: e.reduce_sum(out=den[:], in_=ex[:], axis=AX.X), reads=[ex], writes=[den])
                    k.op("dve", lambda e: e.reciprocal(out=den[:], in_=den[:]), reads=[den], writes=[den])
                    k.ts("dve", G[:, bi, :], ex[:], den[:, 0:1], None, ALU.mult, reads=[ex, den], writes=[G])
                for e in range(lim):
                    sl = []
                    for j in range(6):
                        t_ = slabs[scnt % NS]
                        scnt += 1
                        sl.append(t_)
                        for kc in range(8):
                            if j < 4:
                                src_ = I["moe_w_gu"][l, e, kc * 128:(kc + 1) * 128, j * 512:(j + 1) * 512]
                            else:
                                src_ = I["moe_w_down"][l, e, kc * 128:(kc + 1) * 128, (j - 4) * 512:(j - 3) * 512]
                            k.dma("pool", t_[:, kc, :], src_, writes=[t_])
                    for sb0 in range(0, len(blk), 4):
                        nsb = min(4, len(blk) - sb0)
                        N = nsb * 128
                        hr = [hTb.sub(sb0 + a_) for a_ in range(nsb)]
                        for c in range(8):
                            pg, pl = pf(), pf()
                            gs, ls = sl[c // 4], sl[2 + c // 4]
                            cc = (c % 4) * 128
                            for kc in range(8):
                                k.mm(pg, pg[:, 0:N], gs[:, kc, cc:cc + 128], hTb[:, kc, sb0 * 128:sb0 * 128 + N],
                                     start=(kc == 0), stop=(kc == 7), reads=[gs] + hr)
                            for kc in range(8):
                                k.mm(pl, pl[:, 0:N], ls[:, kc, cc:cc + 128], hTb[:, kc, sb0 * 128:sb0 * 128 + N],
                                     start=(kc == 0), stop=(kc == 7), reads=[ls] + hr)
                            k.ts("dve", gc[:, 0:N], pg[:, 0:N], bguT[:, e * 16 + c:e * 16 + c + 1], 7.0, ALU.add, ALU.min, reads=[pg, bguT], writes=[gc])
                            k.act(sgm[:, 0:N], gc[:, 0:N], AF.Sigmoid, reads=[gc], writes=[sgm], scale=1.702)
                            k.act(lc[:, 0:N], pl[:, 0:N], AF.Identity, reads=[pl, bguT], writes=[lc], bias=bguT[:, e * 16 + 8 + c:e * 16 + 8 + c + 1])
                            k.ts("dve", lc[:, 0:N], lc[:, 0:N], 7.0, -7.0, ALU.min, ALU.max, reads=[lc], writes=[lc])
                            k.tt("dve", gc[:, 0:N], gc[:, 0:N], sgm[:, 0:N], ALU.mult, reads=[gc, sgm], writes=[gc])
                            k.stt(actT[:, c, 0:N], lc[:, 0:N], 1.0, gc[:, 0:N], ALU.add, ALU.mult, reads=[gc, lc], writes=[actT])
                        for ti in range(nsb):
                            bi = sb0 + ti
                            for hf in range(2):
                                pd_ = pf()
                                for c in range(8):
                                    k.mm(pd_, pd_[:, 0:512], actT[:, c, ti * 128:(ti + 1) * 128], sl[4 + hf][:, c, :],
                                         start=(c == 0), stop=(c == 7), reads=[actT, sl[4 + hf]])
                                a_ = acc[:, bi, hf * 512:(hf + 1) * 512]
                                if e == 0:
                                    k.ts("dve", a_, pd_[:, 0:512], G[:, bi, e:e + 1], None, ALU.mult, reads=[pd_, G], writes=[acc])
                                else:
                                    k.stt(a_, pd_[:, 0:512], G[:, bi, e:e + 1], a_, ALU.mult, ALU.add, reads=[pd_, G, acc], writes=[acc])
                for bi, i in enumerate(blk):
                    s = 1 if i < 2 else 0
                    p_ = pf()
                    k.tr(p_, p_[0:32, 0:128], G[:, bi, :], identf, reads=[G, cmf])
                    k.cp("dve", GT[:], p_[0:32, 0:128], reads=[p_], writes=[GT])
                    x_ = xt
                    k.dma("sp", x_[:], xres[i * 128:(i + 1) * 128, :], reads=[xres.sub(i)], writes=[x_])
                    g = L["grow"][1][s]
                    for hf in range(2):
                        p2 = pf()
                        k.mm(p2, p2[:, 0:512], GT[:], bd[:, hf * 512:(hf + 1) * 512], reads=[GT, bd])
                        a_ = acc[:, bi, hf * 512:(hf + 1) * 512]
                        k.tt("dve", a_, a_, p2[:, 0:512], ALU.add, reads=[acc, p2], writes=[acc])
                        k.tt("dve", a_, a_, g[:, hf * 512:(hf + 1) * 512], ALU.mult, reads=[acc, g], writes=[acc])
                        k.tt("dve", x_[:, hf * 512:(hf + 1) * 512], x_[:, hf * 512:(hf + 1) * 512], a_, ALU.add, reads=[x_, acc], writes=[x_])
                    if not final:
                        k.dma("sp", xres[i * 128:(i + 1) * 128, :], x_[:], reads=[x_], writes=[xres.sub(i)])
                    else:
                        k.act(junk[:], x_[:], AF.Square, reads=[x_], writes=[junk, ssq], accum_out=ssq[:])
                        k.act(ssq[:], ssq[:], AF.Sqrt, reads=[ssq], writes=[ssq], scale=1.0 / 1024, bias=EPS)
                        k.op("dve", lambda e: e.reciprocal(out=ssq[:], in_=ssq[:]), reads=[ssq], writes=[ssq])
                        k.stt(x_[:], x_[:], ssq[:, 0:1], fng[:], ALU.mult, ALU.mult, reads=[x_, ssq, fng], writes=[x_])
                        k.dma("sp", yout[(i - 2) * 128:(i - 1) * 128, :], x_[:], reads=[x_], writes=[youtT])

    def stage_ret():
        lim = os.environ.get("RET_LIM")
        with k.scope():
            gm1 = k.sbuf("r_lg", [128, 8], F32)
            k.dma("sp", gm1[:], I["ret_decay_logit"][0].rearrange("d h -> (d h)").partition_broadcast(128), writes=[gm1])
            lgam = k.sbuf("r_lgam", [128, 8], F32)
            nlg = k.sbuf("r_nlg", [128, 8], F32)
            k.act(lgam[:], gm1[:], AF.Exp, reads=[gm1], writes=[lgam], scale=-1.0)
            k.act(lgam[:], lgam[:], AF.Ln, reads=[lgam], writes=[lgam], bias=1.0)
            k.cp("dve", nlg[:], lgam[:], reads=[lgam], writes=[nlg])
            k.ts("dve", lgam[:], lgam[:], -1.0, None, ALU.mult, reads=[lgam], writes=[lgam])
            g128 = k.sbuf("r_g128", [128, 8], F32)
            k.act(g128[:], lgam[:], AF.Exp, reads=[lgam], writes=[g128], scale=128.0)
            ngrow = k.sbuf("r_ng", [128, 512], F32)
            Wq = k.sbuf("r_Wq", [128, 8, 256], BF16)
            Wk = k.sbuf("r_Wk", [128, 8, 256], BF16)
            Wqs = k.sbuf("r_Wqs", [128, 8, 256], BF16)
            Wks = k.sbuf("r_Wks", [128, 8, 256], BF16)
            Wv = k.sbuf("r_Wv", [128, 8, 512], BF16)
            Wg = k.sbuf("r_Wg", [128, 8, 512], BF16)
            Dq = k.sbuf("r_Dq", [128, 128], F32)
            Dk = k.sbuf("r_Dk", [128, 128], F32)
            S32 = k.sbuf("r_S32", [128, 2, 512], F32)
            Sbf = k.sbuf("r_Sbf", [128, 2, 512], BF16)
            rp = [k.sbuf(f"r_rp{i}", [128, 2, 2, 128], F32) for i in range(2)]
            qd = k.sbuf("r_qd", [128, 2, 128], BF16)
            ki = k.sbuf("r_ki", [128, 2, 128], BF16)
            kt = k.sbuf("r_kt", [128, 256], BF16)
            tq = k.sbuf("r_tq", [128, 4, 128], F32)
            tq2 = k.sbuf("r_tq2", [128, 4, 128], F32)
            vb = k.sbuf("r_vb", [128, 512], BF16)
            ST = k.sbuf("r_ST", [128, 128], BF16)
            o32 = [k.sbuf(f"r_o{i}", [128, 512], F32) for i in range(2)]
            oin = [k.sbuf(f"r_oi{i}", [128, 512], F32) for i in range(2)]
            sg = k.sbuf("r_sg", [128, 512], F32)
            st6 = k.sbuf("r_st6", [128, 6], F32)
            mv = k.sbuf("r_mv", [128, 2], F32)
            fin = k.sbuf("r_fin", [128, 512], BF16)
            finT = [k.sbuf(f"r_fT{i}", [128, 4, 128], BF16) for i in range(2)]
            wv_ = I["ret_w_in"][0]
            cnt = 0
            for h in range(4):
                for kc in range(8):
                    r0 = kc * 128
                    k.dma("pool", Wq[:, kc, :], wv_[r0:r0 + 128, h * 256:(h + 1) * 256], writes=[Wq])
                    k.dma("pool", Wk[:, kc, :], wv_[r0:r0 + 128, 1024 + h * 256:1024 + (h + 1) * 256], writes=[Wk])
                    for c in range(2):
                        for hf in range(2):
                            k.dma("pool", Wqs[:, kc, c * 128 + hf * 64:c * 128 + (hf + 1) * 64],
                                  wv_[r0:r0 + 128, h * 256 + c * 128 + (1 - hf) * 64:h * 256 + c * 128 + (2 - hf) * 64], writes=[Wqs])
                            k.dma("pool", Wks[:, kc, c * 128 + hf * 64:c * 128 + (hf + 1) * 64],
                                  wv_[r0:r0 + 128, 1024 + h * 256 + c * 128 + (1 - hf) * 64:1024 + h * 256 + c * 128 + (2 - hf) * 64], writes=[Wks])
                    k.dma("pool", Wv[:, kc, :], wv_[r0:r0 + 128, 2048 + h * 512:2048 + (h + 1) * 512], writes=[Wv])
                    k.dma("pool", Wg[:, kc, :], wv_[r0:r0 + 128, 4096 + h * 512:4096 + (h + 1) * 512], writes=[Wg])
                k.dma("sp", ngrow[:], I["ret_norm_g"][0, h * 512:(h + 1) * 512].partition_broadcast(128), writes=[ngrow])
                for d in range(2):
                    order = list(range(NT)) if d == 0 else [1, 0] + list(range(NT - 1, 1, -1))
                    if lim:
                        order = order[:int(lim)]
                    col = d * 4 + h
                    pos = cmf[:, 6, :] if d == 0 else cmf[:, 7, :]
                    k.act(Dq[:], pos, AF.Exp, reads=[cmf, lgam], writes=[Dq], scale=lgam[:, col:col + 1])
                    k.act(Dk[:], pos, AF.Exp, reads=[cmf, nlg], writes=[Dk], scale=nlg[:, col:col + 1])
                    k.memset("dve", S32[:], 0.0, writes=[S32])
                    k.memset("dve", Sbf[:], 0.0, writes=[Sbf])
                    mask = cmb[:, 1 + d, :]
                    for i in order:
                        hr = hreads(i)
                        cnt += 1
                        lat = i >= 2
                        pq = pf()
                        for gi, Wm in enumerate((Wq, Wk)):
                            for c in range(2):
                                for kc in range(8):
                                    k.mm(pq, pq[:, (gi * 2 + c) * 128:(gi * 2 + c + 1) * 128], Wm[:, kc, c * 128:(c + 1) * 128], hTi(kc, i),
                                         start=(kc == 0), stop=(kc == 7), reads=[Wm] + hr)
                        if lat:
                            ps_ = pf()
                            for gi, Wm in enumerate((Wqs, Wks)):
                                for c in range(2):
                                    for kc in range(8):
                                        k.mm(ps_, ps_[:, (gi * 2 + c) * 128:(gi * 2 + c + 1) * 128], Wm[:, kc, c * 128:(c + 1) * 128], hTi(kc, i),
                                             start=(kc == 0), stop=(kc == 7), reads=[Wm] + hr)
                            r_ = rp[cnt % 2]
                            k.dma("sp", r_[:], I["rope"][:, :, :, (i - 2) * 128:(i - 1) * 128].rearrange("ty cs p t -> p ty cs t"), writes=[r_])
                            for gi in range(2):
                                a_ = tq[:, gi * 2:gi * 2 + 2, :]
                                k.tt("dve", a_, pq[:, gi * 256:(gi + 1) * 256].rearrange("p (a b) -> p a b", a=2), r_[:, :, 0, :], ALU.mult, reads=[pq, r_], writes=[tq])
                                b_ = tq2[:, gi * 2:gi * 2 + 2, :]
                                k.tt("dve", b_, ps_[:, gi * 256:(gi + 1) * 256].rearrange("p (a b) -> p a b", a=2), r_[:, :, 1, :], ALU.mult, reads=[ps_, r_], writes=[tq2])
                            k.tt("pool", tq[:], tq[:], tq2[:], ALU.add, reads=[tq, tq2], writes=[tq])
                        else:
                            k.cp("act", tq[:].rearrange("p a b -> p (a b)"), pq[:, 0:512], reads=[pq], writes=[tq])
                        for c in range(2):
                            k.tt("dve", qd[:, c, :], tq[:, c, :], Dq[:], ALU.mult, reads=[tq, Dq], writes=[qd])
                            k.stt(ki[:, c, :], tq[:, 2 + c, :], 0.0625, Dk[:], ALU.mult, ALU.mult, reads=[tq, Dk], writes=[ki])
                        p_ = pb()
                        for c in range(2):
                            k.tr(p_, p_[:, c * 128:(c + 1) * 128], ki[:, c, :], identb, reads=[ki, cmb])
                        k.cp("act", kt[:], p_[:, 0:256], reads=[p_], writes=[kt])
                        pv = pf()
                        for kc in range(8):
                            k.mm(pv, pv[:, 0:512], hTi(kc, i), Wv[:, kc, :], start=(kc == 0), stop=(kc == 7), reads=[Wv] + hr)
                        k.cp("act", vb[:], pv[:, 0:512], reads=[pv], writes=[vb])
                        if lat:
                            pss = pf()
                            for c in range(2):
                                k.mm(pss, pss[:, 0:128], ki[:, c, :], qd[:, c, :], start=(c == 0), stop=(c == 1), reads=[ki, qd])
                            k.tt("dve", ST[:], pss[:, 0:128], mask, ALU.mult, reads=[pss, cmb], writes=[ST])
                            po = pf()
                            for c in range(2):
                                k.mm(po, po[:, 0:512], qd[:, c, :], Sbf[:, c, :], start=(c == 0), stop=False, reads=[qd, Sbf])
                            k.mm(po, po[:, 0:512], ST[:], vb[:], start=False, stop=True, reads=[ST, vb])
                        for c in range(2):
                            pst = pf()
                            k.mm(pst, pst[:, 0:512], kt[:, c * 128:(c + 1) * 128], vb[:], reads=[kt, vb])
                            k.tt("dve", S32[:, c, :], S32[:, c, :], pst[:, 0:512], ALU.add, reads=[S32, pst], writes=[S32])
                            k.ts("dve", S32[:, c, :], S32[:, c, :], g128[:, col:col + 1], None, ALU.mult, reads=[S32, g128], writes=[S32])
                            k.cp("pool", Sbf[:, c, :], S32[:, c, :], reads=[S32], writes=[Sbf])
                        if not lat:
                            continue
                        o_ = o32[cnt % 2]
                        if d == 0:
                            k.cp("act", o_[:], po[:, 0:512], reads=[po], writes=[o_])
                            k.dma("sp", ofD[i * 128:(i + 1) * 128, h * 512:(h + 1) * 512], o_[:], reads=[o_], writes=[ofD.sub((h, i))])
                        else:
                            oi = oin[cnt % 2]
                            k.dma("sp", oi[:], ofD[i * 128:(i + 1) * 128, h * 512:(h + 1) * 512], reads=[ofD.sub((h, i))], writes=[oi])
                            k.tt("dve", o_[:], po[:, 0:512], oi[:], ALU.add, reads=[po, oi], writes=[o_])
                            k.op("dve", lambda e, o_=o_: e.bn_stats(out=st6[:], in_=o_[:]), reads=[o_], writes=[st6])
                            k.op("dve", lambda e: e.bn_aggr(out=mv[:], in_=st6[:]), reads=[st6], writes=[mv])
                            k.act(mv[:, 1:2], mv[:, 1:2], AF.Sqrt, reads=[mv], writes=[mv], bias=EPS)
                            k.op("dve", lambda e: e.reciprocal(out=mv[:, 1:2], in_=mv[:, 1:2]), reads=[mv], writes=[mv])
                            k.ts("dve", o_[:], o_[:], mv[:, 0:1], mv[:, 1:2], ALU.subtract, ALU.mult, reads=[o_, mv], writes=[o_])
                            pg = pf()
                            for kc in range(8):
                                k.mm(pg, pg[:, 0:512], hTi(kc, i), Wg[:, kc, :], start=(kc == 0), stop=(kc == 7), reads=[Wg] + hr)
                            k.act(sg[:], pg[:, 0:512], AF.Silu, reads=[pg], writes=[sg])
                            k.tt("dve", o_[:], o_[:], ngrow[:], ALU.mult, reads=[o_, ngrow], writes=[o_])
                            k.tt("dve", fin[:], o_[:], sg[:], ALU.mult, reads=[o_, sg], writes=[fin])
                            p_ = pb()
                            for c in range(4):
                                k.tr(p_, p_[:, c * 128:(c + 1) * 128], fin[:, c * 128:(c + 1) * 128], identb, reads=[fin, cmb])
                            ft = finT[cnt % 2]
                            k.cp("act", ft[:].rearrange("p a b -> p (a b)"), p_[:, 0:512], reads=[p_], writes=[ft])
                            k.dma("sp", mixT[h * 512:(h + 1) * 512, i * 128:(i + 1) * 128].rearrange("(c p) t -> p c t", p=128), ft[:],
                                  reads=[ft], writes=[mixT.sub((h, i))])

    def dump_rows(src, r0, nrows, ncols):
        with k.scope():
            t_ = k.sbuf("dump", [128, ncols], F32)
            for a in range(nrows // 128):
                k.dma("sp", t_[:], src[r0 + a * 128:r0 + (a + 1) * 128, 0:ncols], reads=[src] + list(src.subs.values()), writes=[t_])
                k.dma("sp", dbg_out[a * 128:(a + 1) * 128, 0:ncols], t_[:], reads=[t_], writes=[dbgT])

    def alloc_L():
        return {"vecT": k.sbuf("L_vecT", [128, 64], F32), "modF": k.sbuf("L_modF", [128, 48, 2], F32),
                "grow": [[None, None], [k.sbuf(f"L_g1{s}", [128, 1024], F32) for s in range(2)]],
                "mulA": [k.sbuf(f"L_mulA{w}", [128, 8, 2], F32) for w in range(2)]}

    lim_ = os.environ.get("GLA_LIM")
    with k.scope():
        L = alloc_L()
        with k.scope():
            L["grow"][0] = [k.sbuf(f"L_g0{s}", [128, 1024], F32) for s in range(2)]
            stage_mod(0, L)
            with k.scope():
                H["hT"] = k.sbuf("hT", [128, 8, T], BF16)
                pass_a(L, 0, list(range(NT)), xin_T)
                if upto >= 1:
                    stage_gla()
                if upto >= 2:
                    stage_s5()
            if upto >= 3:
                stage_outproj(0, L, "ab_w_out", 8, list(range(NT)) if not lim_ else [0, 1], xin_T, ["g", "s"])
        if upto >= 4:
            mt_ = list(range(NT)) if not os.environ.get("MOE_T") else list(range(int(os.environ["MOE_T"])))
            stage_moe(0, L, mt_, False)
        if dbg is not None:
            if upto == 2 and os.environ.get("S5_DUMP"):
                pass
            elif upto in (1, 2):
                r0 = 0 if upto == 1 else 512
                dc0 = int(os.environ.get("DUMP_C0", "0"))
                with k.scope():
                    t_ = k.sbuf("dump", [128, 4, 256], BF16)
                    t2 = k.sbuf("dump2", [128, 4, 256], F32)
                    k.dma("sp", t_[:], mixT[r0:r0 + 512, dc0:dc0 + 256].rearrange("(c p) t -> p c t", p=128), reads=list(mixT.subs.values()), writes=[t_])
                    k.cp("dve", t2[:], t_[:], reads=[t_], writes=[t2])
                    k.dma("sp", dbg_out, t2[:], reads=[t2], writes=[dbgT])
            elif upto in (3, 4):
                dump_rows(xres, 0, 256 if lim_ else 512, 1024)
    if upto >= 5:
        lat_t = list(range(2, NT))
        with k.scope():
            L = alloc_L()
            with k.scope():
                L["grow"][0] = [k.sbuf(f"L_g0{s}", [128, 1024], F32) for s in range(2)]
                stage_mod(1, L)
                with k.scope():
                    H["hT"] = k.sbuf("hT", [128, 8, T], BF16)
                    pass_a(L, 0, list(range(NT)), xres)
                    stage_ret()
                stage_outproj(1, L, "ret_w_out", 16, lat_t, xres, [0, 1, 2, 3])
            stage_moe(1, L, lat_t, True)
    k.finish()
    nc.used_inputs = list(I.keys())
    return nc


def prep_inputs(inputs, cores=range(8), used=None):
    consts = make_consts()
    maps = []
    for b in cores:
        m = {}
        m["xin"] = np.ascontiguousarray(np.concatenate([inputs["ctx"][b], inputs["x"][b]], axis=0), dtype=np.float32)
        cv = np.stack([np.asarray(inputs["c"][b], np.float32), np.asarray(inputs["c_ctx"], np.float32)], 0)
        m["cvec"] = np.ascontiguousarray(cv.reshape(2, 8, 128).transpose(2, 0, 1).reshape(128, 16))
        m.update(consts)
        for name, _ in PARAMS:
            m[name] = np.ascontiguousarray(inputs[name], dtype=np.float32)
        if used is not None:
            m = {k_: v_ for k_, v_ in m.items() if k_ in used}
        maps.append(m)
    return maps


def kernel(**inputs):
    nc = build()
    maps = prep_inputs(inputs, used=nc.used_inputs)
    res = run_bass_kernel_spmd(nc, maps, core_ids=list(range(8)))
    return np.stack([r["yout"] for r in res.results], 0).astype(np.float32)
```
